# Optimizing a Trainium2 kernel written in Bass

```python
import jax, jax.numpy as jnp
from jax import lax
import numpy as np

D_MODEL = 4096
BATCH = 1
SEQ = 8192
DEPTH = 2
DEC_BATCH = 4
DEC_SEQ = 4096
PAST_LEN = 128

N_MIXERS = 2
N_POOL_LAYERS = (DEPTH + 1) // 2
N_HGRN_LAYERS = DEPTH // 2
N_DENSE_LAYERS = (DEPTH + 1) // 2
N_MOE_LAYERS = DEPTH // 2
POOL_WINDOWS = (2, 4, 8, 16)
POOL_GROUPS = len(POOL_WINDOWS)
POOL_GROUP_DIM = D_MODEL // POOL_GROUPS
HGRN_EXPAND = 128
HGRN_HEADS = D_MODEL // HGRN_EXPAND
HGRN_DK = HGRN_EXPAND
HGRN_DV = D_MODEL // HGRN_HEADS
CHUNK = 16
D_FF = 2 * D_MODEL
N_EXPERTS = 8
TOP_K = 2
MOE_D_FF = D_MODEL // 4
EPS = 1e-6

kernel_name = "hybrid_pool_hgrn2_moe_encoder"


def rms_norm(x, g):
    xf = x.astype(jnp.float32)
    y = xf * lax.rsqrt(jnp.mean(xf * xf, axis=-1, keepdims=True) + EPS)
    return (y * g.astype(jnp.float32)).astype(x.dtype)


def swiglu(h, w_gate, w_up, w_down):
    return (jax.nn.silu(h @ w_gate) * (h @ w_up)) @ w_down


def moe_swiglu(h, router, w_gate, w_up, w_down):
    B, S, D = h.shape
    t = h.reshape(B * S, D)
    logits = (t @ router).astype(jnp.float32)
    vals, idx = lax.top_k(logits, TOP_K)
    gates = jax.nn.softmax(vals, axis=-1)
    combine = jnp.sum(jax.nn.one_hot(idx, N_EXPERTS, dtype=jnp.float32) * gates[..., None], axis=1)
    out = jnp.zeros((B * S, D), jnp.float32)
    for e in range(N_EXPERTS):
        out = out + combine[:, e:e + 1] * swiglu(t, w_gate[e], w_up[e], w_down[e]).astype(jnp.float32)
    return out.astype(h.dtype).reshape(B, S, D)


def pool_mixer(h, w_grp, scale):
    B, S, D = h.shape
    hf = h.astype(jnp.float32)
    csum = jnp.concatenate([jnp.zeros((B, 1, D), jnp.float32), jnp.cumsum(hf, axis=1)], axis=1)
    pos = jnp.arange(S)
    outs = []
    for gi, w in enumerate(POOL_WINDOWS):
        sl = slice(gi * POOL_GROUP_DIM, (gi + 1) * POOL_GROUP_DIM)
        lo = jnp.clip(pos - w // 2, 0, S)
        hi = jnp.clip(pos + w // 2, 0, S)
        cg = csum[:, :, sl]
        cnt = (hi - lo).astype(jnp.float32)
        mean = (cg[:, hi] - cg[:, lo]) / cnt[None, :, None]
        outs.append(mean - hf[:, :, sl])
    d = jnp.stack(outs, axis=2).astype(h.dtype)
    y = jnp.einsum('bsgc,gcd->bsgd', d, w_grp).reshape(B, S, D)
    return y * scale


def gla_chunk_scan(q, k, v, g):
    B, S, H, DK = q.shape
    DV = v.shape[-1]
    n = S // CHUNK

    def to_chunks(a):
        return a.reshape(B, n, CHUNK, H, a.shape[-1]).transpose(1, 0, 3, 2, 4)

    mask = jnp.tril(jnp.ones((CHUNK, CHUNK), bool))[:, :, None]

    def step(state, inp):
        qc, kc, vc, gc = inp
        b = jnp.cumsum(gc, axis=2)
        o_inter = jnp.einsum('bhtd,bhde->bhte', qc * jnp.exp(b), state)
        diff = b[:, :, :, None, :] - b[:, :, None, :, :]
        decay = jnp.where(mask, jnp.exp(jnp.where(mask, diff, 0.0)), 0.0)
        scores = jnp.einsum('bhtd,bhtsd,bhsd->bhts', qc, decay, kc)
        o = o_inter + jnp.einsum('bhts,bhse->bhte', scores, vc)
        b_last = b[:, :, -1:, :]
        state = jnp.exp(b_last[:, :, 0, :])[..., None] * state + jnp.einsum(
            'bhsd,bhse->bhde', kc * jnp.exp(b_last - b), vc)
        return state, o

    state0 = jnp.zeros((B, H, DK, DV), jnp.float32)
    _, o = lax.scan(step, state0, (to_chunks(q), to_chunks(k), to_chunks(v), to_chunks(g)))
    return o.transpose(1, 0, 3, 2, 4).reshape(B, S, H, DV)


def hgrn2_mixer(h, w_in, lb, gain, w_out):
    B, S, D = h.shape
    proj = h @ w_in
    q, f_fw, f_bw, v, g = jnp.split(proj, 5, axis=-1)

    def heads(a):
        return a.astype(jnp.float32).reshape(B, S, HGRN_HEADS, -1)

    q = jax.nn.silu(heads(q))
    v = heads(v)
    lbh = lb.astype(jnp.float32).reshape(HGRN_HEADS, HGRN_DK)

    def forget(fl):
        f = lbh + (1.0 - lbh) * jax.nn.sigmoid(heads(fl))
        return 1.0 - f, jnp.log(f)

    k_fw, g_fw = forget(f_fw)
    k_bw, g_bw = forget(f_bw)
    o_fw = gla_chunk_scan(q, k_fw, v, g_fw)

    def rev(a):
        return jnp.flip(a, axis=1)

    o_bw = rev(gla_chunk_scan(rev(q), rev(k_bw), rev(v), rev(g_bw)))
    o = (o_fw + o_bw).reshape(B, S, D).astype(h.dtype)
    o = rms_norm(o, gain) * jax.nn.silu(g)
    return o @ w_out


def trunk(x, norm_mix, norm_ffn, norm_final, pool_w, pool_scale, hgrn_w_in, lb_all,
          hgrn_norm, hgrn_w_out, ffn_w_gate, ffn_w_up, ffn_w_down,
          moe_router, moe_w_gate, moe_w_up, moe_w_down):
    for i in range(DEPTH):
        j = i // N_MIXERS
        h = rms_norm(x, norm_mix[i])
        if i % N_MIXERS == 0:
            x = x + pool_mixer(h, pool_w[j], pool_scale[j])
        else:
            x = x + hgrn2_mixer(h, hgrn_w_in[j], lb_all[i], hgrn_norm[j], hgrn_w_out[j])
        h = rms_norm(x, norm_ffn[i])
        if i % 2 == 0:
            x = x + swiglu(h, ffn_w_gate[j], ffn_w_up[j], ffn_w_down[j])
        else:
            x = x + moe_swiglu(h, moe_router[j], moe_w_gate[j], moe_w_up[j], moe_w_down[j])
    return rms_norm(x, norm_final)


def setup_inputs(seed: int = 0) -> dict:
    key = jax.random.key(seed)
    ks = jax.random.split(key, 20)

    def normal(k, shape, scale):
        return jax.random.normal(k, shape, jnp.float32) * scale

    D, G, F, E, FE = D_MODEL, POOL_GROUP_DIM, D_FF, N_EXPERTS, MOE_D_FF
    return {
        "x_prompt": normal(ks[0], (BATCH, SEQ, D), 1.0),
        "x_sample": normal(ks[1], (DEC_BATCH, DEC_SEQ, D), 1.0),
        "norm_mix": 1.0 + normal(ks[2], (DEPTH, D), 0.02),
        "norm_ffn": 1.0 + normal(ks[3], (DEPTH, D), 0.02),
        "norm_final": 1.0 + normal(ks[4], (D,), 0.02),
        "pool_w": normal(ks[5], (N_POOL_LAYERS, POOL_GROUPS, G, G), G ** -0.5),
        "pool_scale": 1.0 + normal(ks[6], (N_POOL_LAYERS, D), 0.02),
        "hgrn_w_in": normal(ks[7], (N_HGRN_LAYERS, D, 5 * D), D ** -0.5),
        "hgrn_lb": normal(ks[8], (DEPTH, HGRN_HEADS * HGRN_DK), 0.1),
        "hgrn_norm": 1.0 + normal(ks[9], (N_HGRN_LAYERS, D), 0.02),
        "hgrn_w_out": normal(ks[10], (N_HGRN_LAYERS, D, D), D ** -0.5),
        "ffn_w_gate": normal(ks[11], (N_DENSE_LAYERS, D, F), D ** -0.5),
        "ffn_w_up": normal(ks[12], (N_DENSE_LAYERS, D, F), D ** -0.5),
        "ffn_w_down": normal(ks[13], (N_DENSE_LAYERS, F, D), F ** -0.5),
        "moe_router": normal(ks[14], (N_MOE_LAYERS, D, E), D ** -0.5),
        "moe_w_gate": normal(ks[15], (N_MOE_LAYERS, E, D, FE), D ** -0.5),
        "moe_w_up": normal(ks[16], (N_MOE_LAYERS, E, D, FE), D ** -0.5),
        "moe_w_down": normal(ks[17], (N_MOE_LAYERS, E, FE, D), FE ** -0.5),
    }


def reference(x_prompt, x_sample, norm_mix, norm_ffn, norm_final, pool_w, pool_scale,
              hgrn_w_in, hgrn_lb, hgrn_norm, hgrn_w_out, ffn_w_gate, ffn_w_up, ffn_w_down,
              moe_router, moe_w_gate, moe_w_up, moe_w_down):
    lb_sm = jax.nn.softmax(hgrn_lb.astype(jnp.float32), axis=0)
    lb_all = jnp.cumsum(lb_sm, axis=0) - lb_sm[0:1]
    y_prompt = trunk(x_prompt, norm_mix, norm_ffn, norm_final, pool_w, pool_scale, hgrn_w_in, lb_all,
                     hgrn_norm, hgrn_w_out, ffn_w_gate, ffn_w_up, ffn_w_down,
                     moe_router, moe_w_gate, moe_w_up, moe_w_down)
    y_sample = trunk(x_sample, norm_mix, norm_ffn, norm_final, pool_w, pool_scale, hgrn_w_in, lb_all,
                     hgrn_norm, hgrn_w_out, ffn_w_gate, ffn_w_up, ffn_w_down,
                     moe_router, moe_w_gate, moe_w_up, moe_w_down)
    return (y_prompt, y_sample)
```

```python
import numpy as np
import ml_dtypes
from contextlib import ExitStack
import concourse.bass as bass
import concourse.mybir as mybir
from concourse.bass_utils import run_bass_kernel_spmd

F32 = mybir.dt.float32
BF16 = mybir.dt.bfloat16
AF = mybir.ActivationFunctionType
ALU = mybir.AluOpType

D = 4096
KC = 32
T = 512
NH = 32
FF = 8192
NE = 8
FE = 1024
EPS = 1e-6
POOL_W = (2, 4, 8, 16)
SAME_ENG_SYNC = True


class Buf:
    __slots__ = ("w", "r", "sem", "cnt", "name")

    def __init__(self, name=""):
        self.w = None
        self.r = []
        self.sem = None
        self.cnt = 0
        self.name = name


class Eng:
    def __init__(self, h, sem, name):
        self.h = h
        self.sem = sem
        self.cnt = 0
        self.waited = {}
        self.name = name


class Ctx:
    def __init__(self, nc, es):
        self.nc = nc
        self.es = es
        self.eng = {}
        for n, h in (("pe", nc.tensor), ("act", nc.scalar), ("dve", nc.vector),
                     ("pool", nc.gpsimd), ("sp", nc.sync)):
            self.eng[n] = Eng(h, es.enter_context(nc.semaphore("e_" + n)), n)
        self.dma_bufs = []
        self.nsem = 5

    def _wait(self, e, evs):
        best = {}
        for ev in evs:
            if ev is None:
                continue
            s, v = ev
            if best.get(id(s), (None, 0))[1] < v:
                best[id(s)] = (s, v)
        for s, v in best.values():
            if s is e.sem and not (SAME_ENG_SYNC and e.name in ("act", "dve", "pool")):
                continue
            if e.waited.get(id(s), 0) >= v:
                continue
            e.h.wait_ge(s, v)
            e.waited[id(s)] = v

    def op(self, en, fn, reads=(), writes=()):
        e = self.eng[en]
        evs = []
        for b in reads:
            evs.append(b.w)
        for b in writes:
            evs.append(b.w)
            evs.extend(b.r)
        self._wait(e, evs)
        ins = fn(e.h)
        e.cnt += 1
        ins.then_inc(e.sem, 1)
        ev = (e.sem, e.cnt)
        for b in writes:
            b.w = ev
            b.r = []
        for b in reads:
            b.r.append(ev)
        return ev

    def dma(self, qn, out, in_, sb, reads=(), writes=()):
        e = self.eng[qn]
        if sb.sem is None:
            sb.sem = self.es.enter_context(self.nc.semaphore("d%d" % self.nsem))
            self.nsem += 1
            self.dma_bufs.append(sb)
        evs = [(sb.sem, sb.cnt)] if sb.cnt else []
        for b in reads:
            evs.append(b.w)
        for b in writes:
            evs.append(b.w)
            evs.extend(b.r)
        self._wait(e, evs)
        e.h.dma_start(out=out, in_=in_).then_inc(sb.sem, 16)
        sb.cnt += 16
        ev = (sb.sem, sb.cnt)
        for b in writes:
            b.w = ev
            b.r = []
        for b in reads:
            b.r.append(ev)
        return ev

    def barrier(self):
        evs = [(e.sem, e.cnt) for e in self.eng.values() if e.cnt]
        evs += [(b.sem, b.cnt) for b in self.dma_bufs if b.cnt]
        for e in self.eng.values():
            self._wait(e, evs)


def build(NT, NTC):
    nc = bass.Bass("TRN2", target_bir_lowering=False)
    NTOK = NT * T

    def din(name, shape, dt=F32):
        return nc.dram_tensor(name, list(shape), dt, kind="ExternalInput").ap()

    def dscr(name, shape, dt):
        return nc.dram_tensor(name, list(shape), dt, kind="Internal").ap()

    x_d = din("x", [NTOK, D])
    xc_d = din("xc", [max(NTC, 1) * T, D])
    xhc_d = din("xhc", [max(NTC, 1) * 16, D])
    pmc_d = din("pmc", [max(NTC, 1) * 4 * 128, 4 * T], BF16)
    phc_d = din("phc", [max(NTC, 1) * 16, 4 * T], BF16)
    wfc_d = din("wfc", [D, D])
    minit_d = din("minit", [128, 2])
    xh_d = din("xh", [NT * 16, D])
    pm_d = din("pm", [NT * 4 * 128, 4 * T], BF16)
    ph_d = din("ph", [NT * 16, 4 * T], BF16)
    cst_d = din("cst", [128, 256])
    cstb_d = din("cstb", [128, 512], BF16)
    rst_d = din("rst", [128, T])
    vec_d = din("vec", [128, 9 * 32])
    gfin_d = din("gfin", [D])
    psc_d = din("psc", [D])
    pool_w = din("pool_w", [4 * 1024, 1024])
    w_in = din("w_in", [D, 5 * D])
    w_out = din("w_out", [D, D])
    wg_d = din("wg", [D, FF])
    wu_d = din("wu", [D, FF])
    wd_d = din("wd", [FF, D])
    rt_d = din("rt", [D, NE])
    mg_d = din("mg", [NE * D, FE])
    mu_d = din("mu", [NE * D, FE])
    md_d = din("md", [NE * FE, D])
    y_d = nc.dram_tensor("y", [NTOK, D], F32, kind="ExternalOutput").ap()

    x1s = dscr("x1s", [NTOK, D], F32)
    sctx = dscr("sctx", [128, NH * 128], F32)
    vs = dscr("vs", [NTOK, D], BF16)
    qs = dscr("qs", [NT * NH * 128, T], BF16)
    gts = dscr("gts", [NT * NH * 128, T], BF16)
    fbs = dscr("fbs", [NT * NH * 128, T], F32)
    ofs = dscr("ofs", [NT * NH * 128, T], F32)

    with ExitStack() as es:
        cx = Ctx(nc, es)
        op, dma, barrier = cx.op, cx.dma, cx.barrier

        def sb(name, shape, dt):
            return es.enter_context(nc.sbuf_tensor("sb_" + name, list(shape), dt))

        wsl = [sb("w%d" % j, [128, KC * 256], BF16) for j in range(2)]
        bw = [Buf("w%d" % j) for j in range(2)]
        misc = sb("misc", [128, 4096], F32)
        bmisc = Buf("misc")
        xt = sb("xt", [128, 4, D], F32)
        bx = [Buf("x%d" % a) for a in range(4)]
        hT = sb("hT", [128, KC, T], BF16)
        bh = Buf("h")
        areg = sb("areg", [128, 8192], F32)
        Sf = sb("Sf", [128, NH, 128], F32)
        Sb = sb("Sb", [128, 4, 128], BF16)
        cst = sb("cst", [128, 256], F32)
        cstb = sb("cstb", [128, 512], BF16)
        rst = sb("rst", [128, T], F32)
        vec = sb("vec", [128, 9 * 32], F32)
        lbv = sb("lbv", [128, 3 * 32], F32)
        stat = sb("stat", [128, 64], F32)
        bcst = Buf("cst")
        epsb = sb("epsb", [128, 1], F32)
        minit = sb("minit", [128, 2], F32)
        bstat = Buf("stat")

        ps = [es.enter_context(nc.psum_tensor("ps%d" % j, [128, T], F32)) for j in range(7)]
        bps = [Buf("ps%d" % j) for j in range(7)]
        psb = es.enter_context(nc.psum_tensor("psb", [128, 1024], BF16))
        bpsb = Buf("psb")
        pi = [0]

        def nb():
            j = pi[0] % 5
            pi[0] += 1
            return ps[j], bps[j]

        ident = cst[:, 0:128]
        ones = cst[:, 128:256]
        identb = cstb[:, 0:128]
        mfw = cstb[:, 128:256]
        mbw = cstb[:, 256:384]

        def vcol(k, c):
            return vec[:, k * 32 + c:k * 32 + c + 1]

        dma("sp", cst[:], cst_d[:, :], bcst, writes=[bcst])
        dma("sp", cstb[:], cstb_d[:, :], bcst, writes=[bcst])
        dma("sp", rst[:], rst_d[:, :], bcst, writes=[bcst])
        dma("sp", vec[:], vec_d[:, :], bcst, writes=[bcst])
        dma("sp", minit[:], minit_d[:, :], bcst, writes=[bcst])
        op("dve", lambda h: h.memset(epsb[:, :], EPS), writes=[bcst])
        op("dve", lambda h: h.tensor_tensor(out=lbv[:, 64:96], in0=vec[:, 6 * 32:7 * 32],
                                            in1=vec[:, 5 * 32:6 * 32], op=ALU.subtract),
           reads=[bcst], writes=[bstat])
        op("act", lambda h: h.activation(out=lbv[:, 0:32], in_=lbv[:, 64:96], func=AF.Sigmoid),
           reads=[bstat], writes=[bstat])
        op("dve", lambda h: h.tensor_scalar(out=lbv[:, 32:64], in0=lbv[:, 0:32], scalar1=-1.0,
                                            scalar2=1.0, op0=ALU.mult, op1=ALU.add),
           reads=[bstat], writes=[bstat])
        barrier()

        wrr = [0]

        def wload(src_ap, kc, ncol, slots=(0, 1)):
            j = slots[wrr[0] % len(slots)]
            wrr[0] += 1
            view = wsl[j][:, 0:kc * ncol].rearrange("p (k n) -> p k n", k=kc)
            dma("pool", view, src_ap.rearrange("(k p) n -> p k n", p=128), bw[j], writes=[bw[j]])
            return view, bw[j]

        def rms_stats(a, junk_ap, col):
            op("act", lambda h: h.activation(out=junk_ap, in_=xt[:, a, :], func=AF.Square,
                                             accum_out=stat[:, col:col + 1]),
               reads=[bx[a]], writes=[bstat, bjunk])
            op("act", lambda h: h.activation(out=stat[:, col:col + 1], in_=stat[:, col:col + 1], func=AF.Ln,
                                             scale=1.0 / D, bias=epsb[:, 0:1]),
               reads=[bstat], writes=[bstat])
            op("act", lambda h: h.activation(out=stat[:, col:col + 1], in_=stat[:, col:col + 1], func=AF.Exp,
                                             scale=-0.5),
               reads=[bstat], writes=[bstat])

        xs = areg[:, 0:4096]
        bxs = Buf("xs")
        bjunk = Buf("junk")
        act_t = areg[:, 4096:8192].bitcast(BF16).rearrange("p (k n) -> p k n", k=16)
        bact = Buf("act")
        junk = areg[:, 4096:8192].bitcast(BF16)[:, 0:4096]

        evi = [0]

        def evac(out_ap, in_ap, reads, writes, scale=None):
            k = evi[0] % 2
            evi[0] += 1
            if k == 0:
                if scale is None:
                    op("act", lambda h: h.activation(out=out_ap, in_=in_ap, func=AF.Copy),
                       reads=reads, writes=writes)
                else:
                    op("act", lambda h: h.activation(out=out_ap, in_=in_ap, func=AF.Copy, scale=scale),
                       reads=reads, writes=writes)
            else:
                if scale is None:
                    op("dve", lambda h: h.tensor_copy(out=out_ap, in_=in_ap), reads=reads, writes=writes)
                else:
                    op("dve", lambda h: h.tensor_scalar(out=out_ap, in0=in_ap, scalar1=scale, scalar2=None,
                                                        op0=ALU.mult), reads=reads, writes=writes)

        def build_hT(gk, junk_ap):
            for a in range(4):
                rms_stats(a, junk_ap, a)
                op("act", lambda h: h.activation(out=xs, in_=xt[:, a, :], func=AF.Copy,
                                                 scale=stat[:, a:a + 1]),
                   reads=[bx[a], bstat], writes=[bxs])
                for c0 in range(0, KC, 4):
                    p, bp = nb()

                    def f(h, p=p, c0=c0):
                        r = None
                        for cc in range(4):
                            r = h.transpose(out=p[:, cc * 128:(cc + 1) * 128],
                                            in_=xs[:, (c0 + cc) * 128:(c0 + cc + 1) * 128], identity=ident)
                        return r
                    op("pe", f, reads=[bxs, bcst], writes=[bp])
                    for cc in range(4):
                        evac(hT[:, c0 + cc, a * 128:(a + 1) * 128], p[:, cc * 128:(cc + 1) * 128],
                             reads=[bp, bcst], writes=[bh], scale=vcol(gk, c0 + cc))

        def mm_group(p, bp, pairs, reads):
            def f(h):
                r = None
                n = len(pairs)
                for i, (l, rr) in enumerate(pairs):
                    r = h.matmul(p, l, rr, start=(i == 0), stop=(i == n - 1))
                return r
            op("pe", f, reads=reads, writes=[bp])

        pi7 = [0]

        def nb7():
            j = pi7[0] % 7
            pi7[0] += 1
            return ps[j], bps[j]

        def tok_proj(src, kc, lhs, lreads, sink):
            hk = kc // 2
            v0, b0 = wload(src[0:hk * 128, :], hk, 512)
            v1, b1 = wload(src[hk * 128:kc * 128, :], kc - hk, 512)
            banks = [nb7() for _ in range(4)]
            for a in range(4):
                p, bp = banks[a]

                def f0(h, p=p, a=a):
                    r = None
                    for k in range(hk):
                        r = h.matmul(p[:, :], lhs(k, a), v0[:, k, :], start=(k == 0), stop=False)
                    return r
                op("pe", f0, reads=[b0] + lreads, writes=[bp])
            for a in range(4):
                p, bp = banks[a]

                def f1(h, p=p, a=a):
                    r = None
                    for k in range(hk, kc):
                        r = h.matmul(p[:, :], lhs(k, a), v1[:, k - hk, :], start=False, stop=(k == kc - 1))
                    return r
                op("pe", f1, reads=[b1] + lreads, writes=[bp])
                sink(a, p, bp)

        def swiglu_down(wg_src, wu_src, wd_src, nf, comb_col=None):
            for fb in range(0, nf, 2):
                wgv, bg = wload(wg_src[:, fb * 128:(fb + 2) * 128], KC, 256)
                wuv, bu = wload(wu_src[:, fb * 128:(fb + 2) * 128], KC, 256)
                for s in range(2):
                    pg, bpg = nb()
                    mm_group(pg[:, :], bpg, [(wgv[:, kc, s * 128:(s + 1) * 128], hT[:, kc, :]) for kc in range(KC)],
                             reads=[bg, bh])
                    pu, bpu = nb()
                    mm_group(pu[:, :], bpu, [(wuv[:, kc, s * 128:(s + 1) * 128], hT[:, kc, :]) for kc in range(KC)],
                             reads=[bu, bh])
                    op("act", lambda h, pg=pg: h.activation(out=sgt, in_=pg[:, :], func=AF.Silu),
                       reads=[bpg], writes=[bsg])
                    op("dve", lambda h, pu=pu, k=fb + s: h.tensor_tensor(out=act_t[:, k, :], in0=sgt, in1=pu[:, :],
                                                                        op=ALU.mult),
                       reads=[bsg, bpu], writes=[bact])
            for ob in range(D // 512):
                def sink(a, p, bp, ob=ob):
                    xo = xt[:, a, ob * 512:(ob + 1) * 512]
                    if comb_col is None:
                        op("dve", lambda h: h.tensor_tensor(out=xo, in0=p[:, :], in1=xo, op=ALU.add),
                           reads=[bp, bx[a]], writes=[bx[a]])
                    else:
                        sc = comb[:, a * 8 + comb_col:a * 8 + comb_col + 1]
                        op("dve", lambda h: h.scalar_tensor_tensor(
                            out=xo, in0=p[:, :], scalar=sc, in1=xo, op0=ALU.mult, op1=ALU.add),
                           reads=[bp, bx[a], bcomb], writes=[bx[a]])
                tok_proj(wd_src[:, ob * 512:(ob + 1) * 512], nf, lambda k, a: act_t[:, k, a * 128:(a + 1) * 128],
                         [bact], sink)

        sgt_t = sb("sgt", [128, T], F32)
        sgt = sgt_t[:, :]
        bsg = Buf("sg")
        comb = sb("comb", [128, 32], F32)
        bcomb = Buf("comb")
        rtmp = sb("rtmp", [128, 64], F32)
        brt = Buf("rt")

        h0 = areg[:, :].bitcast(BF16).rearrange("p (a n) -> p a n", a=4)
        bh0 = Buf("h0")
        pmat = misc[:, :].bitcast(BF16).rearrange("p (g a n) -> p g a n", g=4, a=4)
        hh = wsl[1][0:16, 0:4096]
        phm = wsl[1][0:16, 4096:6144]
        bhh = bw[1]
        dT = hT

        def tf(u):
            return areg[:, u * 512:(u + 1) * 512]

        def tb(u, half):
            return areg[:, u * 512:(u + 1) * 512].bitcast(BF16)[:, half * 512:(half + 1) * 512]

        t_sig, t_g, t_k, t_b, t_eb, t_enb, t_o, t_fb = (tf(u) for u in range(8))
        t_qe, t_ke = tb(8, 0), tb(8, 1)
        t_q = [tb(9, 0), tb(9, 1), tb(10, 0), tb(10, 1)]
        t_gate = tb(11, 0)
        t_ketok = [tb(11, 1)[:, 0:128], tb(11, 1)[:, 128:256]]
        t_am = [tb(11, 1)[:, 256:384], tb(11, 1)[:, 384:512]]
        t_ntot = tf(12)[:, 0:8]
        t_z = tf(13)
        t_sq = tf(14)
        names = ["sig", "g", "k", "b", "eb", "enb", "o", "fb", "qe", "ke", "q0", "q1", "q2", "q3", "gate",
                 "kt0", "kt1", "am0", "am1", "ntot", "z", "sq"]
        B = {n: Buf(n) for n in names}
        bS = Buf("S")
        bSb = [Buf("Sb%d" % j) for j in range(4)]
        vtok = xt[:, :, :].rearrange("p a n -> p (a n)").bitcast(BF16)[:, 0:4 * D].rearrange(
            "p (a n) -> p a n", a=4)

        def scan_chunks(hh_, hs, qe, ke, eb, chunk_order, mask, o_ps, bo_ps, fwd):
            for n_, j in enumerate(chunk_order):
                cs = slice(j * 128, (j + 1) * 128)
                kk = n_ % 2
                op("pe", lambda h: h.transpose(out=psb[:, 0:128], in_=ke[:, cs], identity=identb),
                   reads=[B["ke"], bcst], writes=[bpsb])
                op("dve", lambda h: h.tensor_copy(out=t_ketok[kk], in_=psb[:, 0:128]),
                   reads=[bpsb], writes=[B["kt%d" % kk]])
                pa, bpa = nb()
                op("pe", lambda h: h.matmul(pa[:, 0:128], ke[:, cs], qe[:, cs], start=True, stop=True),
                   reads=[B["ke"], B["qe"]], writes=[bpa])
                op("dve", lambda h: h.tensor_tensor(out=t_am[kk], in0=pa[:, 0:128], in1=mask, op=ALU.mult),
                   reads=[bpa, bcst], writes=[B["am%d" % kk]])
                vch = vtok[:, j, hh_ * 128:(hh_ + 1) * 128]

                def fo(h):
                    h.matmul(o_ps[:, cs], Sb[:, hs, :], qe[:, cs], start=True, stop=False)
                    return h.matmul(o_ps[:, cs], vch, t_am[kk], start=False, stop=True)
                op("pe", fo, reads=[bSb[hs], B["qe"], bx[0], bx[1], bx[2], bx[3], B["am%d" % kk]], writes=[bo_ps])
                pu, bpu = nb()
                op("pe", lambda h: h.matmul(pu[:, 0:128], t_ketok[kk], vch, start=True, stop=True),
                   reads=[B["kt%d" % kk], bx[0], bx[1], bx[2], bx[3]], writes=[bpu])
                ecol = eb[:, j * 128 + 127:j * 128 + 128] if fwd else eb[:, j * 128:j * 128 + 1]
                op("dve", lambda h: h.tensor_tensor(out=Sf[:, hh_, :], in0=pu[:, 0:128], in1=Sf[:, hh_, :],
                                                    op=ALU.add),
                   reads=[bpu, bS], writes=[bS])
                op("dve", lambda h: h.tensor_scalar(out=Sf[:, hh_, :], in0=Sf[:, hh_, :], scalar1=ecol,
                                                    scalar2=None, op0=ALU.mult),
                   reads=[bS, B["eb"]], writes=[bS])
                op("act", lambda h: h.activation(out=Sb[:, hs, :], in_=Sf[:, hh_, :], func=AF.Copy),
                   reads=[bS], writes=[bSb[hs]])

        def vproj():
            for vb in range(D // 512):
                def sink(a, p, bp, vb=vb):
                    evac(vtok[:, a, vb * 512:(vb + 1) * 512], p[:, :], reads=[bp], writes=[bx[a]])
                tok_proj(w_in[:, 3 * D + vb * 512:3 * D + (vb + 1) * 512], KC,
                         lambda k, a: hT[:, k, a * 128:(a + 1) * 128], [bh], sink)

        def f_from_psum(p, bp, out_f, bout):
            pass

        op("dve", lambda h: h.memset(Sf[:, :, :].rearrange("p a n -> p (a n)"), 0.0), writes=[bS])

        def layer0(i, xsrc, xhsrc, pmsrc, phsrc):
            r0 = i * T
            for a in range(4):
                dma("sp", xt[:, a, :], xsrc[r0 + a * 128:r0 + (a + 1) * 128, :], bx[a], writes=[bx[a]])
            dma("pool", hh, xhsrc[i * 16:(i + 1) * 16, :], bhh, writes=[bhh])
            dma("sp", phm, phsrc[i * 16:(i + 1) * 16, :], bhh, writes=[bhh])
            dma("sp", pmat, pmsrc[i * 512:(i + 1) * 512, :].rearrange("(g p) (a n) -> p g a n", p=128, a=4),
                bmisc, writes=[bmisc])
            op("act", lambda h: h.activation(out=h0[0:16, 3, :], in_=hh, func=AF.Square,
                                             accum_out=stat[0:16, 8:9]),
               reads=[bhh], writes=[bstat, bh0, bjunk])
            op("act", lambda h: h.activation(out=stat[0:16, 8:9], in_=stat[0:16, 8:9], func=AF.Ln,
                                             scale=1.0 / D, bias=epsb[0:16, 0:1]),
               reads=[bstat], writes=[bstat])
            op("act", lambda h: h.activation(out=stat[0:16, 8:9], in_=stat[0:16, 8:9], func=AF.Exp, scale=-0.5),
               reads=[bstat], writes=[bstat])
            op("act", lambda h: h.activation(out=hh, in_=hh, func=AF.Copy, scale=stat[0:16, 8:9]),
               reads=[bstat, bhh], writes=[bhh])
            for a in range(4):
                rms_stats(a, h0[:, a, :], a)
                op("act", lambda h, a=a: h.activation(out=h0[:, a, :], in_=xt[:, a, :], func=AF.Copy,
                                                      scale=stat[:, a:a + 1]),
                   reads=[bx[a], bstat, bjunk], writes=[bh0, bjunk])
            for c in range(KC):
                g = c // 8
                p, bp = nb()
                pairs = [(h0[:, a, c * 128:(c + 1) * 128], pmat[:, g, a, :]) for a in range(4)]
                pairs.append((hh[:, c * 128:(c + 1) * 128], phm[:, g * 512:(g + 1) * 512]))
                mm_group(p[:, :], bp, pairs, reads=[bh0, bmisc, bhh])
                evac(dT[:, c, :], p[:, :], reads=[bp, bcst], writes=[bh], scale=vcol(0, c))
            barrier()
            dma("sp", xs, psc_d.partition_broadcast(128), bxs, writes=[bxs, bh0])
            for g in range(4):
                for ob in range(4):
                    wv, bwv = wload(pool_w[g * 1024:(g + 1) * 1024, ob * 256:(ob + 1) * 256], 8, 256, slots=(0,))
                    for a in range(4):
                        p, bp = nb()
                        mm_group(p[:, 0:256], bp,
                                 [(dT[:, g * 8 + k, a * 128:(a + 1) * 128], wv[:, k, :]) for k in range(8)],
                                 reads=[bwv, bh])
                        col = g * 1024 + ob * 256
                        op("dve", lambda h, p=p, col=col: h.tensor_tensor(out=sgt[:, 0:256], in0=p[:, 0:256],
                                                                        in1=xs[:, col:col + 256], op=ALU.mult),
                           reads=[bp, bxs], writes=[bsg])
                        op("dve", lambda h, a=a, col=col: h.tensor_tensor(out=xt[:, a, col:col + 256],
                                                                        in0=sgt[:, 0:256],
                                                                        in1=xt[:, a, col:col + 256], op=ALU.add),
                           reads=[bsg, bx[a]], writes=[bx[a]])
            barrier()
            build_hT(2, junk)
            for q in range(4):
                cs = slice(q * 2048, (q + 1) * 2048)
                swiglu_down(wg_d[:, cs], wu_d[:, cs], wd_d[cs, :], 16)


        for i in range(NTC):
            layer0(i, xc_d, xhc_d, pmc_d, phc_d)
            barrier()
            build_hT(1, junk)
            barrier()
            vproj()
            for hb in range(NH // 2):
                wv, bwv = wload(wfc_d[:, hb * 256:(hb + 1) * 256], KC, 256)
                for s_ in range(2):
                    hd = 2 * hb + s_
                    p, bp = nb()
                    mm_group(p[:, :], bp, [(wv[:, kc, s_ * 128:(s_ + 1) * 128], hT[:, kc, :]) for kc in range(KC)],
                             reads=[bwv, bh])
                    op("act", lambda h: h.activation(out=t_sig, in_=p[:, :], func=AF.Sigmoid),
                       reads=[bp], writes=[B["sig"]])
                    op("dve", lambda h: h.tensor_scalar(out=t_k, in0=t_sig, scalar1=lbv[:, 32 + hd:33 + hd],
                                                        scalar2=lbv[:, hd:hd + 1], op0=ALU.mult, op1=ALU.add),
                       reads=[B["sig"], bstat], writes=[B["k"]])
                    op("act", lambda h: h.activation(out=t_g, in_=t_k, func=AF.Ln), reads=[B["k"]], writes=[B["g"]])
                    op("dve", lambda h: h.tensor_scalar(out=t_k, in0=t_k, scalar1=-1.0, scalar2=1.0,
                                                        op0=ALU.mult, op1=ALU.add),
                       reads=[B["k"], B["g"]], writes=[B["k"]])
                    op("dve", lambda h: h.tensor_tensor_scan(out=t_b, data0=rst[:, :], data1=t_g, initial=0.0,
                                                             op0=ALU.mult, op1=ALU.add),
                       reads=[B["g"], bcst], writes=[B["b"]])
                    op("act", lambda h: h.activation(out=t_eb, in_=t_b, func=AF.Exp), reads=[B["b"]], writes=[B["eb"]])
                    op("act", lambda h: h.activation(out=t_enb, in_=t_b, func=AF.Exp, scale=-1.0),
                       reads=[B["b"]], writes=[B["enb"]])
                    op("dve", lambda h: h.tensor_tensor(out=t_ke, in0=t_k, in1=t_enb, op=ALU.mult),
                       reads=[B["k"], B["enb"]], writes=[B["ke"]])
                    for j in range(4):
                        cs = slice(j * 128, (j + 1) * 128)
                        kk = j % 2
                        op("pe", lambda h: h.transpose(out=psb[:, 0:128], in_=t_ke[:, cs], identity=identb),
                           reads=[B["ke"], bcst], writes=[bpsb])
                        op("dve", lambda h: h.tensor_copy(out=t_ketok[kk], in_=psb[:, 0:128]),
                           reads=[bpsb], writes=[B["kt%d" % kk]])
                        vch = vtok[:, j, hd * 128:(hd + 1) * 128]
                        pu, bpu = nb()
                        op("pe", lambda h: h.matmul(pu[:, 0:128], t_ketok[kk], vch, start=True, stop=True),
                           reads=[B["kt%d" % kk], bx[0], bx[1], bx[2], bx[3]], writes=[bpu])
                        op("dve", lambda h: h.tensor_tensor(out=Sf[:, hd, :], in0=pu[:, 0:128], in1=Sf[:, hd, :],
                                                            op=ALU.add), reads=[bpu, bS], writes=[bS])
                        op("dve", lambda h: h.tensor_scalar(out=Sf[:, hd, :], in0=Sf[:, hd, :],
                                                            scalar1=t_eb[:, j * 128 + 127:j * 128 + 128],
                                                            scalar2=None, op0=ALU.mult),
                           reads=[bS, B["eb"]], writes=[bS])
            barrier()
        Sflat = Sf[:, :, :].rearrange("p a n -> p (a n)")
        dma("sp", sctx[:, :], Sflat, bS, reads=[bS])
        op("dve", lambda h: h.tensor_scalar(out=Sflat, in0=Sflat, scalar1=minit[:, 0:1], scalar2=None, op0=ALU.mult),
           reads=[bS, bcst], writes=[bS])
        barrier()


        for i in range(NT):
            r0 = i * T
            layer0(i, x_d, xh_d, pm_d, ph_d)
            for a in range(4):
                dma("sp", x1s[r0 + a * 128:r0 + (a + 1) * 128, :], xt[:, a, :], bx[a], reads=[bx[a]])
            barrier()
            build_hT(1, junk)
            barrier()
            vproj()
            for a in range(4):
                dma("sp", vs[r0 + a * 128:r0 + (a + 1) * 128, :], vtok[:, a, :], bx[a], reads=[bx[a]])
            for hb in range(NH // 2):
                heads = (2 * hb, 2 * hb + 1)
                srow = [(i * NH + hd) * 128 for hd in heads]
                wv, bwv = wload(w_in[:, hb * 256:(hb + 1) * 256], KC, 256)
                for s, hd in enumerate(heads):
                    p, bp = nb()
                    mm_group(p[:, :], bp, [(wv[:, kc, s * 128:(s + 1) * 128], hT[:, kc, :]) for kc in range(KC)],
                             reads=[bwv, bh])
                    op("act", lambda h, p=p, s=s: h.activation(out=t_q[s], in_=p[:, :], func=AF.Silu),
                       reads=[bp], writes=[B["q%d" % s]])
                    dma("sp", qs[srow[s]:srow[s] + 128, :], t_q[s], B["q%d" % s], reads=[B["q%d" % s]])
                wv, bwv = wload(w_in[:, 4 * D + hb * 256:4 * D + (hb + 1) * 256], KC, 256)
                for s, hd in enumerate(heads):
                    p, bp = nb()
                    mm_group(p[:, :], bp, [(wv[:, kc, s * 128:(s + 1) * 128], hT[:, kc, :]) for kc in range(KC)],
                             reads=[bwv, bh])
                    op("act", lambda h, p=p: h.activation(out=t_gate, in_=p[:, :], func=AF.Silu),
                       reads=[bp], writes=[B["gate"]])
                    dma("sp", gts[srow[s]:srow[s] + 128, :], t_gate, B["gate"], reads=[B["gate"]])
                wv, bwv = wload(w_in[:, 2 * D + hb * 256:2 * D + (hb + 1) * 256], KC, 256)
                for s, hd in enumerate(heads):
                    p, bp = nb()
                    mm_group(p[:, :], bp, [(wv[:, kc, s * 128:(s + 1) * 128], hT[:, kc, :]) for kc in range(KC)],
                             reads=[bwv, bh])
                    op("act", lambda h, p=p: h.activation(out=t_sig, in_=p[:, :], func=AF.Sigmoid),
                       reads=[bp], writes=[B["sig"]])
                    op("dve", lambda h, hd=hd: h.tensor_scalar(out=t_fb, in0=t_sig, scalar1=lbv[:, 32 + hd:33 + hd],
                                                               scalar2=lbv[:, hd:hd + 1], op0=ALU.mult, op1=ALU.add),
                       reads=[B["sig"], bstat], writes=[B["fb"]])
                    dma("sp", fbs[srow[s]:srow[s] + 128, :], t_fb, B["fb"], reads=[B["fb"]])
                wv, bwv = wload(w_in[:, D + hb * 256:D + (hb + 1) * 256], KC, 256)
                for s, hd in enumerate(heads):
                    p, bp = nb()
                    mm_group(p[:, :], bp, [(wv[:, kc, s * 128:(s + 1) * 128], hT[:, kc, :]) for kc in range(KC)],
                             reads=[bwv, bh])
                    op("act", lambda h, p=p: h.activation(out=t_sig, in_=p[:, :], func=AF.Sigmoid),
                       reads=[bp], writes=[B["sig"]])
                    op("dve", lambda h, hd=hd: h.tensor_scalar(out=t_k, in0=t_sig, scalar1=lbv[:, 32 + hd:33 + hd],
                                                               scalar2=lbv[:, hd:hd + 1], op0=ALU.mult, op1=ALU.add),
                       reads=[B["sig"], bstat], writes=[B["k"]])
                    op("act", lambda h: h.activation(out=t_g, in_=t_k, func=AF.Ln),
                       reads=[B["k"]], writes=[B["g"]])
                    op("dve", lambda h: h.tensor_scalar(out=t_k, in0=t_k, scalar1=-1.0, scalar2=1.0,
                                                        op0=ALU.mult, op1=ALU.add),
                       reads=[B["k"], B["g"]], writes=[B["k"]])
                    op("dve", lambda h: h.tensor_tensor_scan(out=t_b, data0=rst[:, :], data1=t_g, initial=0.0,
                                                             op0=ALU.mult, op1=ALU.add),
                       reads=[B["g"], bcst], writes=[B["b"]])
                    op("act", lambda h: h.activation(out=t_eb, in_=t_b, func=AF.Exp),
                       reads=[B["b"]], writes=[B["eb"]])
                    op("act", lambda h: h.activation(out=t_enb, in_=t_b, func=AF.Exp, scale=-1.0),
                       reads=[B["b"]], writes=[B["enb"]])
                    op("dve", lambda h, s=s: h.tensor_tensor(out=t_qe, in0=t_q[s], in1=t_eb, op=ALU.mult),
                       reads=[B["q%d" % s], B["eb"]], writes=[B["qe"]])
                    op("dve", lambda h: h.tensor_tensor(out=t_ke, in0=t_k, in1=t_enb, op=ALU.mult),
                       reads=[B["k"], B["enb"]], writes=[B["ke"]])
                    op("act", lambda h, hd=hd, s=s: h.activation(out=Sb[:, s, :], in_=Sf[:, hd, :], func=AF.Copy),
                       reads=[bS], writes=[bSb[s]])
                    po, bpo = ps[6], bps[6]
                    scan_chunks(hd, s, t_qe, t_ke, t_eb, (0, 1, 2, 3), mfw, po, bpo, True)
                    op("act", lambda h, po=po: h.activation(out=t_o, in_=po[:, :], func=AF.Copy),
                       reads=[bpo], writes=[B["o"]])
                    dma("sp", ofs[srow[s]:srow[s] + 128, :], t_o, B["o"], reads=[B["o"]])
            barrier()

        dma("sp", Sflat, sctx[:, :], bS, writes=[bS])
        op("dve", lambda h: h.tensor_scalar(out=Sflat, in0=Sflat, scalar1=minit[:, 1:2], scalar2=None, op0=ALU.mult),
           reads=[bS, bcst], writes=[bS])
        dma("sp", misc[:, :], gfin_d.partition_broadcast(128), bmisc, writes=[bmisc])
        oT = hT
        for i in reversed(range(NT)):
            r0 = i * T
            for a in range(4):
                dma("sp", vtok[:, a, :], vs[r0 + a * 128:r0 + (a + 1) * 128, :], bx[a], writes=[bx[a]])
            pss, bpss = ps[5], bps[5]
            for hd in range(NH):
                srow = (i * NH + hd) * 128
                s = hd % 4
                dma("sp", t_q[0], qs[srow:srow + 128, :], B["q0"], writes=[B["q0"]])
                dma("sp", t_fb, fbs[srow:srow + 128, :], B["fb"], writes=[B["fb"]])
                dma("sp", t_o, ofs[srow:srow + 128, :], B["o"], writes=[B["o"]])
                op("act", lambda h: h.activation(out=t_g, in_=t_fb, func=AF.Ln), reads=[B["fb"]], writes=[B["g"]])
                op("dve", lambda h: h.tensor_scalar(out=t_k, in0=t_fb, scalar1=-1.0, scalar2=1.0,
                                                    op0=ALU.mult, op1=ALU.add), reads=[B["fb"]], writes=[B["k"]])
                op("dve", lambda h: h.tensor_tensor_scan(out=t_b, data0=rst[:, :], data1=t_g, initial=0.0,
                                                         op0=ALU.mult, op1=ALU.add),
                   reads=[B["g"], bcst], writes=[B["b"]])
                op("dve", lambda h: h.tensor_tensor(out=t_z, in0=t_g, in1=t_b, op=ALU.subtract),
                   reads=[B["g"], B["b"]], writes=[B["z"]])
                op("dve", lambda h: h.tensor_scalar(
                    out=t_ntot[:, 0:4], in0=t_b.rearrange("p (j n) -> p j n", j=4)[:, :, 127], scalar1=-1.0,
                    scalar2=None, op0=ALU.mult), reads=[B["b"]], writes=[B["ntot"]])
                for j in range(4):
                    cs = slice(j * 128, (j + 1) * 128)
                    op("act", lambda h, cs=cs, j=j: h.activation(out=t_eb[:, cs], in_=t_z[:, cs], func=AF.Exp,
                                                                 bias=t_b[:, j * 128 + 127:j * 128 + 128]),
                       reads=[B["z"], B["b"]], writes=[B["eb"]])
                    op("act", lambda h, cs=cs, j=j: h.activation(out=t_enb[:, cs], in_=t_z[:, cs], func=AF.Exp,
                                                                 scale=-1.0, bias=t_ntot[:, j:j + 1]),
                       reads=[B["z"], B["ntot"]], writes=[B["enb"]])
                op("dve", lambda h: h.tensor_tensor(out=t_qe, in0=t_q[0], in1=t_eb, op=ALU.mult),
                   reads=[B["q0"], B["eb"]], writes=[B["qe"]])
                op("dve", lambda h: h.tensor_tensor(out=t_ke, in0=t_k, in1=t_enb, op=ALU.mult),
                   reads=[B["k"], B["enb"]], writes=[B["ke"]])
                op("act", lambda h, hd=hd, s=s: h.activation(out=Sb[:, s, :], in_=Sf[:, hd, :], func=AF.Copy),
                   reads=[bS], writes=[bSb[s]])
                po, bpo = ps[6], bps[6]
                scan_chunks(hd, s, t_qe, t_ke, t_eb, (3, 2, 1, 0), mbw, po, bpo, False)
                op("dve", lambda h, po=po: h.tensor_tensor(out=t_o, in0=po[:, :], in1=t_o, op=ALU.add),
                   reads=[bpo, B["o"]], writes=[B["o"]])
                op("act", lambda h: h.activation(out=t_sq, in_=t_o, func=AF.Square), reads=[B["o"]],
                   writes=[B["sq"]])
                op("pe", lambda h, hd=hd: h.matmul(pss[:, :], ones, t_sq, start=(hd == 0), stop=(hd == NH - 1)),
                   reads=[B["sq"], bcst], writes=[bpss])
                op("dve", lambda h, hd=hd: h.tensor_copy(out=oT[:, hd, :], in_=t_o), reads=[B["o"]], writes=[bh])
            op("act", lambda h: h.activation(out=t_sq, in_=pss[:, :], func=AF.Ln, scale=1.0 / D, bias=epsb[:, 0:1]),
               reads=[bpss], writes=[B["sq"]])
            op("act", lambda h: h.activation(out=t_sq, in_=t_sq, func=AF.Exp, scale=-0.5),
               reads=[B["sq"]], writes=[B["sq"]])
            for hd in range(NH):
                srow = (i * NH + hd) * 128
                dma("sp", t_gate, gts[srow:srow + 128, :], B["gate"], writes=[B["gate"]])
                op("dve", lambda h, hd=hd: h.scalar_tensor_tensor(out=t_z, in0=oT[:, hd, :], scalar=vcol(4, hd),
                                                                  in1=t_sq, op0=ALU.mult, op1=ALU.mult),
                   reads=[bh, B["sq"], bcst], writes=[B["z"]])
                op("dve", lambda h, hd=hd: h.tensor_tensor(out=oT[:, hd, :], in0=t_z, in1=t_gate, op=ALU.mult),
                   reads=[B["z"], B["gate"]], writes=[bh])
            barrier()
            for a in range(4):
                dma("sp", xt[:, a, :], x1s[r0 + a * 128:r0 + (a + 1) * 128, :], bx[a], writes=[bx[a]])
            for ob in range(D // 512):
                def sink(a, p, bp, ob=ob):
                    xo = xt[:, a, ob * 512:(ob + 1) * 512]
                    op("dve", lambda h: h.tensor_tensor(out=xo, in0=p[:, :], in1=xo, op=ALU.add),
                       reads=[bp, bx[a]], writes=[bx[a]])
                tok_proj(w_out[:, ob * 512:(ob + 1) * 512], KC, lambda k, a: oT[:, k, a * 128:(a + 1) * 128],
                         [bh], sink)
            barrier()
            build_hT(3, junk)
            wv, bwv = wload(rt_d[:, :], KC, NE)
            for a in range(4):
                p, bp = nb()
                mm_group(p[:, 0:NE], bp, [(hT[:, kc, a * 128:(a + 1) * 128], wv[:, kc, :]) for kc in range(KC)],
                         reads=[bwv, bh])
                lg = rtmp[:, 0:8]
                op("dve", lambda h, p=p: h.tensor_copy(out=lg, in_=p[:, 0:NE]), reads=[bp], writes=[brt])
                op("dve", lambda h: h.max(out=rtmp[:, 8:16], in_=lg), reads=[brt], writes=[brt])
                op("dve", lambda h: h.tensor_tensor(out=rtmp[:, 16:17], in0=rtmp[:, 8:9], in1=rtmp[:, 9:10],
                                                    op=ALU.subtract), reads=[brt], writes=[brt])
                op("act", lambda h: h.activation(out=rtmp[:, 17:18], in_=rtmp[:, 16:17], func=AF.Sigmoid),
                   reads=[brt], writes=[brt])
                op("act", lambda h: h.activation(out=rtmp[:, 18:19], in_=rtmp[:, 16:17], func=AF.Sigmoid,
                                                 scale=-1.0), reads=[brt], writes=[brt])
                op("dve", lambda h: h.tensor_scalar(out=rtmp[:, 24:32], in0=lg, scalar1=rtmp[:, 8:9],
                                                    scalar2=rtmp[:, 17:18], op0=ALU.is_equal, op1=ALU.mult),
                   reads=[brt], writes=[brt])
                op("dve", lambda h: h.tensor_scalar(out=rtmp[:, 32:40], in0=lg, scalar1=rtmp[:, 9:10],
                                                    scalar2=rtmp[:, 18:19], op0=ALU.is_equal, op1=ALU.mult),
                   reads=[brt], writes=[brt])
                op("dve", lambda h, a=a: h.tensor_tensor(out=comb[:, a * 8:(a + 1) * 8], in0=rtmp[:, 24:32],
                                                         in1=rtmp[:, 32:40], op=ALU.add),
                   reads=[brt, bcomb], writes=[bcomb])
            for e in range(NE):
                swiglu_down(mg_d[e * D:(e + 1) * D, :], mu_d[e * D:(e + 1) * D, :], md_d[e * FE:(e + 1) * FE, :],
                            8, comb_col=e)
            barrier()
            for a in range(4):
                rms_stats(a, junk, a)
                op("dve", lambda h, a=a: h.scalar_tensor_tensor(out=xs, in0=xt[:, a, :], scalar=stat[:, a:a + 1],
                                                                in1=misc[:, :], op0=ALU.mult, op1=ALU.mult),
                   reads=[bx[a], bstat, bmisc], writes=[bxs])
                dma("sp", y_d[r0 + a * 128:r0 + (a + 1) * 128, :], xs, bxs, reads=[bxs])
            barrier()
        barrier()
    return nc


_NC_CACHE = {}


def _pool_mats(pos_main, pos_halo, L):
    NT = pos_main.shape[0]
    pm = np.zeros((NT, 4, 4, 128, T), np.float32)
    ph = np.zeros((NT, 4, 16, T), np.float32)
    for i in range(NT):
        pt = pos_main[i].astype(np.int64)
        R = np.concatenate([pos_halo[i, 0:8], pos_main[i], pos_halo[i, 8:16]]).astype(np.int64)
        okr = (R >= 0) & (R < L)
        okt = (pt >= 0) & (pt < L)
        for g, w in enumerate(POOL_W):
            lo = np.clip(pt - w // 2, 0, L)
            hi = np.clip(pt + w // 2, 0, L)
            cnt = (hi - lo).astype(np.float32)
            inv = np.where(okt & (cnt > 0), 1.0 / np.maximum(cnt, 1.0), 0.0).astype(np.float32)
            M = ((R[:, None] >= lo[None, :]) & (R[:, None] < hi[None, :]) & okr[:, None]).astype(np.float32) * inv[None, :]
            M -= ((R[:, None] == pt[None, :]) & okt[None, :] & okr[:, None]).astype(np.float32)
            pm[i, g] = M[8:8 + T].reshape(4, 128, T)
            ph[i, g, 0:8] = M[0:8]
            ph[i, g, 8:16] = M[8 + T:]
    pm2 = np.ascontiguousarray(pm.transpose(0, 1, 3, 2, 4)).reshape(NT * 4 * 128, 4 * T)
    ph2 = np.ascontiguousarray(ph.transpose(0, 2, 1, 3)).reshape(NT * 16, 4 * T)
    return pm2.astype(ml_dtypes.bfloat16), ph2.astype(ml_dtypes.bfloat16)


def _consts():
    cst = np.zeros((128, 256), np.float32)
    cst[:, 0:128] = np.eye(128, dtype=np.float32)
    cst[:, 128:256] = 1.0
    cb = np.zeros((128, 512), np.float32)
    cb[:, 0:128] = np.eye(128)
    s = np.arange(128)[:, None]
    t = np.arange(128)[None, :]
    cb[:, 128:256] = (s <= t)
    cb[:, 256:384] = (s >= t)
    rst = np.ones((128, T), np.float32)
    rst[:, 0::128] = 0.0
    return cst, cb.astype(ml_dtypes.bfloat16), rst


def _vec_layout(v):
    return np.ascontiguousarray(np.asarray(v, np.float32).reshape(32, 128).T)


BIG = -(1 << 40)


def _gather(seq, pos):
    L = seq.shape[0]
    out = np.zeros((pos.size, D), np.float32)
    ok = (pos >= 0) & (pos < L)
    out[ok.reshape(-1)] = seq[pos.reshape(-1)[ok.reshape(-1)]]
    return out


def _tiles(pos):
    NT = pos.size // T
    main = pos.reshape(NT, T)
    halo = np.full((NT, 16), BIG, np.int64)
    for i in range(NT):
        m = main[i]
        if m[0] < 0:
            continue
        lo_, hi_ = m.min(), m.max()
        halo[i, 0:8] = lo_ - 8 + np.arange(8)
        halo[i, 8:16] = hi_ + 1 + np.arange(8)
    return main, halo


def _run(jobs, NT, NTC, norm_mix, norm_ffn, norm_final, pool_w, pool_scale, hgrn_w_in, hgrn_lb, hgrn_norm,
         hgrn_w_out, ffn_w_gate, ffn_w_up, ffn_w_down, moe_router, moe_w_gate, moe_w_up, moe_w_down):
    key = (NT, NTC)
    if key not in _NC_CACHE:
        _NC_CACHE[key] = build(NT, NTC)
    nc = _NC_CACHE[key]
    cst, cstb, rst = _consts()
    vec = np.zeros((128, 9 * 32), np.float32)
    for k, v in enumerate([norm_mix[0], norm_mix[1], norm_ffn[0], norm_ffn[1], hgrn_norm[0], hgrn_lb[0], hgrn_lb[1]]):
        vec[:, k * 32:(k + 1) * 32] = _vec_layout(v)
    w_in = np.ascontiguousarray(hgrn_w_in[0])
    wf = {1: np.ascontiguousarray(w_in[:, D:2 * D]), 2: np.ascontiguousarray(w_in[:, 2 * D:3 * D])}
    wf[0] = wf[1]
    shared = {
        "cst": cst, "cstb": cstb, "rst": rst, "vec": vec,
        "gfin": np.ascontiguousarray(norm_final, np.float32),
        "psc": np.ascontiguousarray(pool_scale[0], np.float32),
        "pool_w": np.ascontiguousarray(pool_w[0].reshape(4 * 1024, 1024)),
        "w_in": w_in, "w_out": np.ascontiguousarray(hgrn_w_out[0]),
        "wg": np.ascontiguousarray(ffn_w_gate[0]), "wu": np.ascontiguousarray(ffn_w_up[0]),
        "wd": np.ascontiguousarray(ffn_w_down[0]), "rt": np.ascontiguousarray(moe_router[0]),
        "mg": np.ascontiguousarray(moe_w_gate[0].reshape(NE * D, FE)),
        "mu": np.ascontiguousarray(moe_w_up[0].reshape(NE * D, FE)),
        "md": np.ascontiguousarray(moe_w_down[0].reshape(NE * FE, D)),
    }
    in_maps = []
    zero_seq = np.zeros((0, D), np.float32)
    cache = {}
    for seq, own_pos, ctx_pos, cdir in jobs:
        if seq is None:
            seq = zero_seq
        L = seq.shape[0]
        m = dict(shared)
        om, oh = _tiles(own_pos)
        cm, ch = _tiles(ctx_pos)
        m["x"] = _gather(seq, om)
        m["xh"] = _gather(seq, oh)
        m["xc"] = _gather(seq, cm)
        m["xhc"] = _gather(seq, ch)
        ko = ("o", L, own_pos[0], own_pos[-1])
        if ko not in cache:
            cache[ko] = _pool_mats(om, oh, L)
        m["pm"], m["ph"] = cache[ko]
        kc_ = ("c", L, ctx_pos[0], ctx_pos[-1])
        if kc_ not in cache:
            cache[kc_] = _pool_mats(cm, ch, L)
        m["pmc"], m["phc"] = cache[kc_]
        m["wfc"] = wf[cdir]
        mi = np.zeros((128, 2), np.float32)
        if cdir == 1:
            mi[:, 0] = 1.0
        elif cdir == 2:
            mi[:, 1] = 1.0
        m["minit"] = mi
        in_maps.append(m)
    res = run_bass_kernel_spmd(nc, in_maps, core_ids=list(range(8)))
    return [res.results[c]["y"] for c in range(8)]


def kernel(x_prompt, x_sample, **w):
    x_prompt = np.asarray(x_prompt)
    x_sample = np.asarray(x_sample)
    w = {k: np.asarray(v) for k, v in w.items()}
    LP = x_prompt.shape[1]
    LS = x_sample.shape[1]
    half = LP // 2
    NT = half // T
    NTC = half // T
    assert LS == half and x_sample.shape[0] == 4 and x_prompt.shape[0] == 1
    none_pos = np.full(NTC * T, BIG, np.int64)
    jobs = [
        (x_prompt[0], np.arange(0, half), np.arange(LP - 1, half - 1, -1), 2),
        (x_prompt[0], np.arange(half, LP), np.arange(0, half), 1),
    ]
    for b in range(4):
        jobs.append((x_sample[b], np.arange(0, LS), none_pos, 0))
    while len(jobs) < 8:
        jobs.append((None, np.full(NT * T, BIG, np.int64), none_pos, 0))
    ys = _run(jobs, NT, NTC, **w)
    yp = np.concatenate([ys[0], ys[1]], 0)[None]
    ysamp = np.stack([ys[2 + b] for b in range(4)], 0)
    return (np.ascontiguousarray(yp, dtype=np.float32), np.ascontiguousarray(ysamp, dtype=np.float32))
```

```python
import numpy as np
import ml_dtypes
from contextlib import ExitStack
import concourse.bass as bass
import concourse.mybir as mybir
from concourse.bass_utils import run_bass_kernel_spmd

F32 = mybir.dt.float32
BF16 = mybir.dt.bfloat16
AF = mybir.ActivationFunctionType
ALU = mybir.AluOpType

D = 4096
KC = 32
T = 512
NH = 32
FF = 8192
NE = 8
FE = 1024
EPS = 1e-6
POOL_W = (2, 4, 8, 16)
SAME_ENG_SYNC = True


class Buf:
    __slots__ = ("w", "r", "sem", "cnt", "name")

    def __init__(self, name=""):
        self.w = None
        self.r = []
        self.sem = None
        self.cnt = 0
        self.name = name


class Eng:
    def __init__(self, h, sem, name):
        self.h = h
        self.sem = sem
        self.cnt = 0
        self.waited = {}
        self.name = name


class Ctx:
    def __init__(self, nc, es):
        self.nc = nc
        self.es = es
        self.eng = {}
        for n, h in (("pe", nc.tensor), ("act", nc.scalar), ("dve", nc.vector),
                     ("pool", nc.gpsimd), ("sp", nc.sync)):
            self.eng[n] = Eng(h, es.enter_context(nc.semaphore("e_" + n)), n)
        self.dma_bufs = []
        self.nsem = 5

    def _wait(self, e, evs):
        best = {}
        for ev in evs:
            if ev is None:
                continue
            s, v = ev
            if best.get(id(s), (None, 0))[1] < v:
                best[id(s)] = (s, v)
        for s, v in best.values():
            if s is e.sem and not (SAME_ENG_SYNC and e.name in ("act", "dve", "pool")):
                continue
            if e.waited.get(id(s), 0) >= v:
                continue
            e.h.wait_ge(s, v)
            e.waited[id(s)] = v

    def op(self, en, fn, reads=(), writes=()):
        e = self.eng[en]
        evs = []
        for b in reads:
            evs.append(b.w)
        for b in writes:
            evs.append(b.w)
            evs.extend(b.r)
        self._wait(e, evs)
        ins = fn(e.h)
        e.cnt += 1
        ins.then_inc(e.sem, 1)
        ev = (e.sem, e.cnt)
        for b in writes:
            b.w = ev
            b.r = []
        for b in reads:
            b.r.append(ev)
        return ev

    def dma(self, qn, out, in_, sb, reads=(), writes=()):
        e = self.eng[qn]
        if sb.sem is None:
            sb.sem = self.es.enter_context(self.nc.semaphore("d%d" % self.nsem))
            self.nsem += 1
            self.dma_bufs.append(sb)
        evs = [(sb.sem, sb.cnt)] if sb.cnt else []
        for b in reads:
            evs.append(b.w)
        for b in writes:
            evs.append(b.w)
            evs.extend(b.r)
        self._wait(e, evs)
        e.h.dma_start(out=out, in_=in_).then_inc(sb.sem, 16)
        sb.cnt += 16
        ev = (sb.sem, sb.cnt)
        for b in writes:
            b.w = ev
            b.r = []
        for b in reads:
            b.r.append(ev)
        return ev

    def barrier(self):
        evs = [(e.sem, e.cnt) for e in self.eng.values() if e.cnt]
        evs += [(b.sem, b.cnt) for b in self.dma_bufs if b.cnt]
        for e in self.eng.values():
            self._wait(e, evs)


def build(NT, NTC):
    nc = bass.Bass("TRN2", target_bir_lowering=False)
    NTOK = NT * T

    def din(name, shape, dt=F32):
        return nc.dram_tensor(name, list(shape), dt, kind="ExternalInput").ap()

    def dscr(name, shape, dt):
        return nc.dram_tensor(name, list(shape), dt, kind="Internal").ap()

    x_d = din("x", [NTOK, D])
    xc_d = din("xc", [max(NTC, 1) * T, D])
    xhc_d = din("xhc", [max(NTC, 1) * 16, D])
    pmc_d = din("pmc", [max(NTC, 1) * 4 * 128, 4 * T], BF16)
    phc_d = din("phc", [max(NTC, 1) * 16, 4 * T], BF16)
    wfc_d = din("wfc", [D, D])
    minit_d = din("minit", [128, 2])
    xh_d = din("xh", [NT * 16, D])
    pm_d = din("pm", [NT * 4 * 128, 4 * T], BF16)
    ph_d = din("ph", [NT * 16, 4 * T], BF16)
    cst_d = din("cst", [128, 256])
    cstb_d = din("cstb", [128, 512], BF16)
    rst_d = din("rst", [128, T])
    vec_d = din("vec", [128, 9 * 32])
    gfin_d = din("gfin", [D])
    psc_d = din("psc", [D])
    pool_w = din("pool_w", [4 * 1024, 1024])
    w_in = din("w_in", [D, 5 * D])
    w_out = din("w_out", [D, D])
    wg_d = din("wg", [D, FF])
    wu_d = din("wu", [D, FF])
    wd_d = din("wd", [FF, D])
    rt_d = din("rt", [D, NE])
    mg_d = din("mg", [NE * D, FE])
    mu_d = din("mu", [NE * D, FE])
    md_d = din("md", [NE * FE, D])
    y_d = nc.dram_tensor("y", [NTOK, D], F32, kind="ExternalOutput").ap()

    x1s = dscr("x1s", [NTOK, D], F32)
    sctx = dscr("sctx", [128, NH * 128], F32)
    vs = dscr("vs", [NTOK, D], BF16)
    qs = dscr("qs", [NT * NH * 128, T], BF16)
    gts = dscr("gts", [NT * NH * 128, T], BF16)
    fbs = dscr("fbs", [NT * NH * 128, T], F32)
    ofs = dscr("ofs", [NT * NH * 128, T], F32)

    with ExitStack() as es:
        cx = Ctx(nc, es)
        op, dma, barrier = cx.op, cx.dma, cx.barrier

        def sb(name, shape, dt):
            return es.enter_context(nc.sbuf_tensor("sb_" + name, list(shape), dt))

        wsl = [sb("w%d" % j, [128, KC * 256], BF16) for j in range(3)]
        bw = [Buf("w%d" % j) for j in range(3)]
        bmisc = bw[2]
        xt = sb("xt", [128, 4, D], F32)
        bx = [Buf("x%d" % a) for a in range(4)]
        hT = sb("hT", [128, KC, T], BF16)
        bh = Buf("h")
        areg = sb("areg", [128, 8192], F32)
        Sf = sb("Sf", [128, NH, 128], F32)
        Sb = sb("Sb", [128, 4, 128], BF16)
        cst = sb("cst", [128, 256], F32)
        cstb = sb("cstb", [128, 512], BF16)
        rst = sb("rst", [128, T], F32)
        vec = sb("vec", [128, 9 * 32], F32)
        lbv = sb("lbv", [128, 3 * 32], F32)
        stat = sb("stat", [128, 64], F32)
        bcst = Buf("cst")
        epsb = sb("epsb", [128, 1], F32)
        minit = sb("minit", [128, 2], F32)
        bstat = Buf("stat")

        ps = [es.enter_context(nc.psum_tensor("ps%d" % j, [128, T], F32)) for j in range(7)]
        bps = [Buf("ps%d" % j) for j in range(7)]
        psb = es.enter_context(nc.psum_tensor("psb", [128, 1024], BF16))
        bpsb = Buf("psb")
        pi = [0]

        def nb():
            j = pi[0] % 5
            pi[0] += 1
            return ps[j], bps[j]

        ident = cst[:, 0:128]
        ones = cst[:, 128:256]
        identb = cstb[:, 0:128]
        mfw = cstb[:, 128:256]
        mbw = cstb[:, 256:384]

        def vcol(k, c):
            return vec[:, k * 32 + c:k * 32 + c + 1]

        dma("sp", cst[:], cst_d[:, :], bcst, writes=[bcst])
        dma("sp", cstb[:], cstb_d[:, :], bcst, writes=[bcst])
        dma("sp", rst[:], rst_d[:, :], bcst, writes=[bcst])
        dma("sp", vec[:], vec_d[:, :], bcst, writes=[bcst])
        dma("sp", minit[:], minit_d[:, :], bcst, writes=[bcst])
        op("dve", lambda h: h.memset(epsb[:, :], EPS), writes=[bcst])
        op("dve", lambda h: h.tensor_tensor(out=lbv[:, 64:96], in0=vec[:, 6 * 32:7 * 32],
                                            in1=vec[:, 5 * 32:6 * 32], op=ALU.subtract),
           reads=[bcst], writes=[bstat])
        op("act", lambda h: h.activation(out=lbv[:, 0:32], in_=lbv[:, 64:96], func=AF.Sigmoid),
           reads=[bstat], writes=[bstat])
        op("dve", lambda h: h.tensor_scalar(out=lbv[:, 32:64], in0=lbv[:, 0:32], scalar1=-1.0,
                                            scalar2=1.0, op0=ALU.mult, op1=ALU.add),
           reads=[bstat], writes=[bstat])
        barrier()

        wrr = [0]

        def wload(src_ap, kc, ncol, slots=(0, 1, 2)):
            j = slots[wrr[0] % len(slots)]
            wrr[0] += 1
            view = wsl[j][:, 0:kc * ncol].rearrange("p (k n) -> p k n", k=kc)
            dma("pool", view, src_ap.rearrange("(k p) n -> p k n", p=128), bw[j], writes=[bw[j]])
            return view, bw[j]

        def rms_stats(a, junk_ap, col):
            op("act", lambda h: h.activation(out=junk_ap, in_=xt[:, a, :], func=AF.Square,
                                             accum_out=stat[:, col:col + 1]),
               reads=[bx[a]], writes=[bstat, bjunk])
            op("act", lambda h: h.activation(out=stat[:, col:col + 1], in_=stat[:, col:col + 1], func=AF.Ln,
                                             scale=1.0 / D, bias=epsb[:, 0:1]),
               reads=[bstat], writes=[bstat])
            op("act", lambda h: h.activation(out=stat[:, col:col + 1], in_=stat[:, col:col + 1], func=AF.Exp,
                                             scale=-0.5),
               reads=[bstat], writes=[bstat])

        xs = areg[:, 0:4096]
        bxs = Buf("xs")
        bjunk = Buf("junk")
        act_t = areg[:, 4096:8192].bitcast(BF16).rearrange("p (k n) -> p k n", k=16)
        bact = Buf("act")
        junk = areg[:, 4096:8192].bitcast(BF16)[:, 0:4096]

        evi = [0]

        def evac(out_ap, in_ap, reads, writes, scale=None):
            k = evi[0] % 2
            evi[0] += 1
            if k == 0:
                if scale is None:
                    op("act", lambda h: h.activation(out=out_ap, in_=in_ap, func=AF.Copy),
                       reads=reads, writes=writes)
                else:
                    op("act", lambda h: h.activation(out=out_ap, in_=in_ap, func=AF.Copy, scale=scale),
                       reads=reads, writes=writes)
            else:
                if scale is None:
                    op("dve", lambda h: h.tensor_copy(out=out_ap, in_=in_ap), reads=reads, writes=writes)
                else:
                    op("dve", lambda h: h.tensor_scalar(out=out_ap, in0=in_ap, scalar1=scale, scalar2=None,
                                                        op0=ALU.mult), reads=reads, writes=writes)

        def build_hT(gk, junk_ap):
            for a in range(4):
                rms_stats(a, junk_ap, a)
                op("act", lambda h: h.activation(out=xs, in_=xt[:, a, :], func=AF.Copy,
                                                 scale=stat[:, a:a + 1]),
                   reads=[bx[a], bstat], writes=[bxs])
                for c0 in range(0, KC, 4):
                    p, bp = nb()

                    def f(h, p=p, c0=c0):
                        r = None
                        for cc in range(4):
                            r = h.transpose(out=p[:, cc * 128:(cc + 1) * 128],
                                            in_=xs[:, (c0 + cc) * 128:(c0 + cc + 1) * 128], identity=ident)
                        return r
                    op("pe", f, reads=[bxs, bcst], writes=[bp])
                    for cc in range(4):
                        evac(hT[:, c0 + cc, a * 128:(a + 1) * 128], p[:, cc * 128:(cc + 1) * 128],
                             reads=[bp, bcst], writes=[bh], scale=vcol(gk, c0 + cc))

        def mm_group(p, bp, pairs, reads):
            def f(h):
                r = None
                n = len(pairs)
                for i, (l, rr) in enumerate(pairs):
                    r = h.matmul(p, l, rr, start=(i == 0), stop=(i == n - 1))
                return r
            op("pe", f, reads=reads, writes=[bp])

        pi7 = [0]

        def nb7():
            j = pi7[0] % 7
            pi7[0] += 1
            return ps[j], bps[j]

        def tok_proj(src, kc, lhs, lreads, sink):
            hk = kc // 2
            v0, b0 = wload(src[0:hk * 128, :], hk, 512)
            v1, b1 = wload(src[hk * 128:kc * 128, :], kc - hk, 512)
            banks = [nb7() for _ in range(4)]
            for a in range(4):
                p, bp = banks[a]

                def f0(h, p=p, a=a):
                    r = None
                    for k in range(hk):
                        r = h.matmul(p[:, :], lhs(k, a), v0[:, k, :], start=(k == 0), stop=False)
                    return r
                op("pe", f0, reads=[b0] + lreads, writes=[bp])
            for a in range(4):
                p, bp = banks[a]

                def f1(h, p=p, a=a):
                    r = None
                    for k in range(hk, kc):
                        r = h.matmul(p[:, :], lhs(k, a), v1[:, k - hk, :], start=False, stop=(k == kc - 1))
                    return r
                op("pe", f1, reads=[b1] + lreads, writes=[bp])
                sink(a, p, bp)

        def swiglu_down(wg_src, wu_src, wd_src, nf, comb_col=None):
            for fb in range(0, nf, 2):
                wgv, bg = wload(wg_src[:, fb * 128:(fb + 2) * 128], KC, 256)
                wuv, bu = wload(wu_src[:, fb * 128:(fb + 2) * 128], KC, 256)
                for s in range(2):
                    pg, bpg = nb()
                    mm_group(pg[:, :], bpg, [(wgv[:, kc, s * 128:(s + 1) * 128], hT[:, kc, :]) for kc in range(KC)],
                             reads=[bg, bh])
                    pu, bpu = nb()
                    mm_group(pu[:, :], bpu, [(wuv[:, kc, s * 128:(s + 1) * 128], hT[:, kc, :]) for kc in range(KC)],
                             reads=[bu, bh])
                    op("act", lambda h, pg=pg: h.activation(out=sgt, in_=pg[:, :], func=AF.Silu),
                       reads=[bpg], writes=[bsg])
                    op("dve", lambda h, pu=pu, k=fb + s: h.tensor_tensor(out=act_t[:, k, :], in0=sgt, in1=pu[:, :],
                                                                        op=ALU.mult),
                       reads=[bsg, bpu], writes=[bact])
            for ob in range(D // 512):
                def sink(a, p, bp, ob=ob):
                    xo = xt[:, a, ob * 512:(ob + 1) * 512]
                    if comb_col is None:
                        op("dve", lambda h: h.tensor_tensor(out=xo, in0=p[:, :], in1=xo, op=ALU.add),
                           reads=[bp, bx[a]], writes=[bx[a]])
                    else:
                        sc = comb[:, a * 8 + comb_col:a * 8 + comb_col + 1]
                        op("dve", lambda h: h.scalar_tensor_tensor(
                            out=xo, in0=p[:, :], scalar=sc, in1=xo, op0=ALU.mult, op1=ALU.add),
                           reads=[bp, bx[a], bcomb], writes=[bx[a]])
                tok_proj(wd_src[:, ob * 512:(ob + 1) * 512], nf, lambda k, a: act_t[:, k, a * 128:(a + 1) * 128],
                         [bact], sink)

        sgt_t = sb("sgt", [128, T], F32)
        sgt = sgt_t[:, :]
        bsg = Buf("sg")
        comb = sb("comb", [128, 32], F32)
        bcomb = Buf("comb")
        rtmp = sb("rtmp", [128, 64], F32)
        brt = Buf("rt")

        h0 = areg[:, :].bitcast(BF16).rearrange("p (a n) -> p a n", a=4)
        bh0 = Buf("h0")
        pmat = wsl[2][:, :].rearrange("p (g a n) -> p g a n", g=4, a=4)
        hh = wsl[1][0:16, 0:4096]
        phm = wsl[1][0:16, 4096:6144]
        bhh = bw[1]
        dT = hT

        def tf(u):
            return areg[:, u * 512:(u + 1) * 512]

        def tb(u, half):
            return areg[:, u * 512:(u + 1) * 512].bitcast(BF16)[:, half * 512:(half + 1) * 512]

        t_sig, t_g, t_k, t_b, t_eb, t_enb, t_o, t_fb = (tf(u) for u in range(8))
        t_qe, t_ke = tb(8, 0), tb(8, 1)
        t_q = [tb(9, 0), tb(9, 1), tb(10, 0), tb(10, 1)]
        t_gate = tb(11, 0)
        t_ketok = [tb(11, 1)[:, 0:128], tb(11, 1)[:, 128:256]]
        t_am = [tb(11, 1)[:, 256:384], tb(11, 1)[:, 384:512]]
        t_ntot = tf(12)[:, 0:8]
        t_z = tf(13)
        t_sq = tf(14)
        names = ["sig", "g", "k", "b", "eb", "enb", "o", "fb", "qe", "ke", "q0", "q1", "q2", "q3", "gate",
                 "kt0", "kt1", "am0", "am1", "ntot", "z", "sq"]
        B = {n: Buf(n) for n in names}
        bS = Buf("S")
        bSb = [Buf("Sb%d" % j) for j in range(4)]
        vtok = xt[:, :, :].rearrange("p a n -> p (a n)").bitcast(BF16)[:, 0:4 * D].rearrange(
            "p (a n) -> p a n", a=4)

        def scan_chunks(hh_, hs, qe, ke, eb, chunk_order, mask, o_ps, bo_ps, fwd):
            for n_, j in enumerate(chunk_order):
                cs = slice(j * 128, (j + 1) * 128)
                kk = n_ % 2
                op("pe", lambda h: h.transpose(out=psb[:, 0:128], in_=ke[:, cs], identity=identb),
                   reads=[B["ke"], bcst], writes=[bpsb])
                op("dve", lambda h: h.tensor_copy(out=t_ketok[kk], in_=psb[:, 0:128]),
                   reads=[bpsb], writes=[B["kt%d" % kk]])
                pa, bpa = nb()
                op("pe", lambda h: h.matmul(pa[:, 0:128], ke[:, cs], qe[:, cs], start=True, stop=True),
                   reads=[B["ke"], B["qe"]], writes=[bpa])
                op("dve", lambda h: h.tensor_tensor(out=t_am[kk], in0=pa[:, 0:128], in1=mask, op=ALU.mult),
                   reads=[bpa, bcst], writes=[B["am%d" % kk]])
                vch = vtok[:, j, hh_ * 128:(hh_ + 1) * 128]

                def fo(h):
                    h.matmul(o_ps[:, cs], Sb[:, hs, :], qe[:, cs], start=True, stop=False)
                    return h.matmul(o_ps[:, cs], vch, t_am[kk], start=False, stop=True)
                op("pe", fo, reads=[bSb[hs], B["qe"], bx[0], bx[1], bx[2], bx[3], B["am%d" % kk]], writes=[bo_ps])
                pu, bpu = nb()
                op("pe", lambda h: h.matmul(pu[:, 0:128], t_ketok[kk], vch, start=True, stop=True),
                   reads=[B["kt%d" % kk], bx[0], bx[1], bx[2], bx[3]], writes=[bpu])
                ecol = eb[:, j * 128 + 127:j * 128 + 128] if fwd else eb[:, j * 128:j * 128 + 1]
                op("dve", lambda h: h.tensor_tensor(out=Sf[:, hh_, :], in0=pu[:, 0:128], in1=Sf[:, hh_, :],
                                                    op=ALU.add),
                   reads=[bpu, bS], writes=[bS])
                op("dve", lambda h: h.tensor_scalar(out=Sf[:, hh_, :], in0=Sf[:, hh_, :], scalar1=ecol,
                                                    scalar2=None, op0=ALU.mult),
                   reads=[bS, B["eb"]], writes=[bS])
                op("act", lambda h: h.activation(out=Sb[:, hs, :], in_=Sf[:, hh_, :], func=AF.Copy),
                   reads=[bS], writes=[bSb[hs]])

        def vproj():
            for vb in range(D // 512):
                def sink(a, p, bp, vb=vb):
                    evac(vtok[:, a, vb * 512:(vb + 1) * 512], p[:, :], reads=[bp], writes=[bx[a]])
                tok_proj(w_in[:, 3 * D + vb * 512:3 * D + (vb + 1) * 512], KC,
                         lambda k, a: hT[:, k, a * 128:(a + 1) * 128], [bh], sink)

        def f_from_psum(p, bp, out_f, bout):
            pass

        op("dve", lambda h: h.memset(Sf[:, :, :].rearrange("p a n -> p (a n)"), 0.0), writes=[bS])

        def layer0(i, xsrc, xhsrc, pmsrc, phsrc):
            r0 = i * T
            for a in range(4):
                dma("sp", xt[:, a, :], xsrc[r0 + a * 128:r0 + (a + 1) * 128, :], bx[a], writes=[bx[a]])
            dma("pool", hh, xhsrc[i * 16:(i + 1) * 16, :], bhh, writes=[bhh])
            dma("sp", phm, phsrc[i * 16:(i + 1) * 16, :], bhh, writes=[bhh])
            dma("sp", pmat, pmsrc[i * 512:(i + 1) * 512, :].rearrange("(g p) (a n) -> p g a n", p=128, a=4),
                bmisc, writes=[bmisc])
            op("act", lambda h: h.activation(out=h0[0:16, 3, :], in_=hh, func=AF.Square,
                                             accum_out=stat[0:16, 8:9]),
               reads=[bhh], writes=[bstat, bh0, bjunk])
            op("act", lambda h: h.activation(out=stat[0:16, 8:9], in_=stat[0:16, 8:9], func=AF.Ln,
                                             scale=1.0 / D, bias=epsb[0:16, 0:1]),
               reads=[bstat], writes=[bstat])
            op("act", lambda h: h.activation(out=stat[0:16, 8:9], in_=stat[0:16, 8:9], func=AF.Exp, scale=-0.5),
               reads=[bstat], writes=[bstat])
            op("act", lambda h: h.activation(out=hh, in_=hh, func=AF.Copy, scale=stat[0:16, 8:9]),
               reads=[bstat, bhh], writes=[bhh])
            for a in range(4):
                rms_stats(a, h0[:, a, :], a)
                op("act", lambda h, a=a: h.activation(out=h0[:, a, :], in_=xt[:, a, :], func=AF.Copy,
                                                      scale=stat[:, a:a + 1]),
                   reads=[bx[a], bstat, bjunk], writes=[bh0, bjunk])
            for c in range(KC):
                g = c // 8
                p, bp = nb()
                pairs = [(h0[:, a, c * 128:(c + 1) * 128], pmat[:, g, a, :]) for a in range(4)]
                pairs.append((hh[:, c * 128:(c + 1) * 128], phm[:, g * 512:(g + 1) * 512]))
                mm_group(p[:, :], bp, pairs, reads=[bh0, bmisc, bhh])
                evac(dT[:, c, :], p[:, :], reads=[bp, bcst], writes=[bh], scale=vcol(0, c))
            barrier()
            dma("sp", xs, psc_d.partition_broadcast(128), bxs, writes=[bxs, bh0])
            for g in range(4):
                for ob in range(4):
                    wv, bwv = wload(pool_w[g * 1024:(g + 1) * 1024, ob * 256:(ob + 1) * 256], 8, 256, slots=(0,))
                    for a in range(4):
                        p, bp = nb()
                        mm_group(p[:, 0:256], bp,
                                 [(dT[:, g * 8 + k, a * 128:(a + 1) * 128], wv[:, k, :]) for k in range(8)],
                                 reads=[bwv, bh])
                        col = g * 1024 + ob * 256
                        op("dve", lambda h, p=p, col=col: h.tensor_tensor(out=sgt[:, 0:256], in0=p[:, 0:256],
                                                                        in1=xs[:, col:col + 256], op=ALU.mult),
                           reads=[bp, bxs], writes=[bsg])
                        op("dve", lambda h, a=a, col=col: h.tensor_tensor(out=xt[:, a, col:col + 256],
                                                                        in0=sgt[:, 0:256],
                                                                        in1=xt[:, a, col:col + 256], op=ALU.add),
                           reads=[bsg, bx[a]], writes=[bx[a]])
            barrier()
            build_hT(2, junk)
            for q in range(4):
                cs = slice(q * 2048, (q + 1) * 2048)
                swiglu_down(wg_d[:, cs], wu_d[:, cs], wd_d[cs, :], 16)


        for i in range(NTC):
            layer0(i, xc_d, xhc_d, pmc_d, phc_d)
            barrier()
            build_hT(1, junk)
            barrier()
            vproj()
            for hb in range(NH // 2):
                wv, bwv = wload(wfc_d[:, hb * 256:(hb + 1) * 256], KC, 256)
                for s_ in range(2):
                    hd = 2 * hb + s_
                    p, bp = nb()
                    mm_group(p[:, :], bp, [(wv[:, kc, s_ * 128:(s_ + 1) * 128], hT[:, kc, :]) for kc in range(KC)],
                             reads=[bwv, bh])
                    op("act", lambda h: h.activation(out=t_sig, in_=p[:, :], func=AF.Sigmoid),
                       reads=[bp], writes=[B["sig"]])
                    op("dve", lambda h: h.tensor_scalar(out=t_k, in0=t_sig, scalar1=lbv[:, 32 + hd:33 + hd],
                                                        scalar2=lbv[:, hd:hd + 1], op0=ALU.mult, op1=ALU.add),
                       reads=[B["sig"], bstat], writes=[B["k"]])
                    op("act", lambda h: h.activation(out=t_g, in_=t_k, func=AF.Ln), reads=[B["k"]], writes=[B["g"]])
                    op("dve", lambda h: h.tensor_scalar(out=t_k, in0=t_k, scalar1=-1.0, scalar2=1.0,
                                                        op0=ALU.mult, op1=ALU.add),
                       reads=[B["k"], B["g"]], writes=[B["k"]])
                    op("dve", lambda h: h.tensor_tensor_scan(out=t_b, data0=rst[:, :], data1=t_g, initial=0.0,
                                                             op0=ALU.mult, op1=ALU.add),
                       reads=[B["g"], bcst], writes=[B["b"]])
                    op("act", lambda h: h.activation(out=t_eb, in_=t_b, func=AF.Exp), reads=[B["b"]], writes=[B["eb"]])
                    op("act", lambda h: h.activation(out=t_enb, in_=t_b, func=AF.Exp, scale=-1.0),
                       reads=[B["b"]], writes=[B["enb"]])
                    op("dve", lambda h: h.tensor_tensor(out=t_ke, in0=t_k, in1=t_enb, op=ALU.mult),
                       reads=[B["k"], B["enb"]], writes=[B["ke"]])
                    for j in range(4):
                        cs = slice(j * 128, (j + 1) * 128)
                        kk = j % 2
                        op("pe", lambda h: h.transpose(out=psb[:, 0:128], in_=t_ke[:, cs], identity=identb),
                           reads=[B["ke"], bcst], writes=[bpsb])
                        op("dve", lambda h: h.tensor_copy(out=t_ketok[kk], in_=psb[:, 0:128]),
                           reads=[bpsb], writes=[B["kt%d" % kk]])
                        vch = vtok[:, j, hd * 128:(hd + 1) * 128]
                        pu, bpu = nb()
                        op("pe", lambda h: h.matmul(pu[:, 0:128], t_ketok[kk], vch, start=True, stop=True),
                           reads=[B["kt%d" % kk], bx[0], bx[1], bx[2], bx[3]], writes=[bpu])
                        op("dve", lambda h: h.tensor_tensor(out=Sf[:, hd, :], in0=pu[:, 0:128], in1=Sf[:, hd, :],
                                                            op=ALU.add), reads=[bpu, bS], writes=[bS])
                        op("dve", lambda h: h.tensor_scalar(out=Sf[:, hd, :], in0=Sf[:, hd, :],
                                                            scalar1=t_eb[:, j * 128 + 127:j * 128 + 128],
                                                            scalar2=None, op0=ALU.mult),
                           reads=[bS, B["eb"]], writes=[bS])
            barrier()
        Sflat = Sf[:, :, :].rearrange("p a n -> p (a n)")
        dma("sp", sctx[:, :], Sflat, bS, reads=[bS])
        op("dve", lambda h: h.tensor_scalar(out=Sflat, in0=Sflat, scalar1=minit[:, 0:1], scalar2=None, op0=ALU.mult),
           reads=[bS, bcst], writes=[bS])
        barrier()


        for i in range(NT):
            r0 = i * T
            layer0(i, x_d, xh_d, pm_d, ph_d)
            for a in range(4):
                dma("sp", x1s[r0 + a * 128:r0 + (a + 1) * 128, :], xt[:, a, :], bx[a], reads=[bx[a]])
            barrier()
            build_hT(1, junk)
            barrier()
            vproj()
            for a in range(4):
                dma("sp", vs[r0 + a * 128:r0 + (a + 1) * 128, :], vtok[:, a, :], bx[a], reads=[bx[a]])
            for hb in range(NH // 2):
                heads = (2 * hb, 2 * hb + 1)
                srow = [(i * NH + hd) * 128 for hd in heads]
                wv, bwv = wload(w_in[:, hb * 256:(hb + 1) * 256], KC, 256)
                for s, hd in enumerate(heads):
                    p, bp = nb()
                    mm_group(p[:, :], bp, [(wv[:, kc, s * 128:(s + 1) * 128], hT[:, kc, :]) for kc in range(KC)],
                             reads=[bwv, bh])
                    op("act", lambda h, p=p, s=s: h.activation(out=t_q[s], in_=p[:, :], func=AF.Silu),
                       reads=[bp], writes=[B["q%d" % s]])
                    dma("sp", qs[srow[s]:srow[s] + 128, :], t_q[s], B["q%d" % s], reads=[B["q%d" % s]])
                wv, bwv = wload(w_in[:, 4 * D + hb * 256:4 * D + (hb + 1) * 256], KC, 256)
                for s, hd in enumerate(heads):
                    p, bp = nb()
                    mm_group(p[:, :], bp, [(wv[:, kc, s * 128:(s + 1) * 128], hT[:, kc, :]) for kc in range(KC)],
                             reads=[bwv, bh])
                    op("act", lambda h, p=p: h.activation(out=t_gate, in_=p[:, :], func=AF.Silu),
                       reads=[bp], writes=[B["gate"]])
                    dma("sp", gts[srow[s]:srow[s] + 128, :], t_gate, B["gate"], reads=[B["gate"]])
                wv, bwv = wload(w_in[:, 2 * D + hb * 256:2 * D + (hb + 1) * 256], KC, 256)
                for s, hd in enumerate(heads):
                    p, bp = nb()
                    mm_group(p[:, :], bp, [(wv[:, kc, s * 128:(s + 1) * 128], hT[:, kc, :]) for kc in range(KC)],
                             reads=[bwv, bh])
                    op("act", lambda h, p=p: h.activation(out=t_sig, in_=p[:, :], func=AF.Sigmoid),
                       reads=[bp], writes=[B["sig"]])
                    op("dve", lambda h, hd=hd: h.tensor_scalar(out=t_fb, in0=t_sig, scalar1=lbv[:, 32 + hd:33 + hd],
                                                               scalar2=lbv[:, hd:hd + 1], op0=ALU.mult, op1=ALU.add),
                       reads=[B["sig"], bstat], writes=[B["fb"]])
                    dma("sp", fbs[srow[s]:srow[s] + 128, :], t_fb, B["fb"], reads=[B["fb"]])
                wv, bwv = wload(w_in[:, D + hb * 256:D + (hb + 1) * 256], KC, 256)
                for s, hd in enumerate(heads):
                    p, bp = nb()
                    mm_group(p[:, :], bp, [(wv[:, kc, s * 128:(s + 1) * 128], hT[:, kc, :]) for kc in range(KC)],
                             reads=[bwv, bh])
                    op("act", lambda h, p=p: h.activation(out=t_sig, in_=p[:, :], func=AF.Sigmoid),
                       reads=[bp], writes=[B["sig"]])
                    op("dve", lambda h, hd=hd: h.tensor_scalar(out=t_k, in0=t_sig, scalar1=lbv[:, 32 + hd:33 + hd],
                                                               scalar2=lbv[:, hd:hd + 1], op0=ALU.mult, op1=ALU.add),
                       reads=[B["sig"], bstat], writes=[B["k"]])
                    op("act", lambda h: h.activation(out=t_g, in_=t_k, func=AF.Ln),
                       reads=[B["k"]], writes=[B["g"]])
                    op("dve", lambda h: h.tensor_scalar(out=t_k, in0=t_k, scalar1=-1.0, scalar2=1.0,
                                                        op0=ALU.mult, op1=ALU.add),
                       reads=[B["k"], B["g"]], writes=[B["k"]])
                    op("dve", lambda h: h.tensor_tensor_scan(out=t_b, data0=rst[:, :], data1=t_g, initial=0.0,
                                                             op0=ALU.mult, op1=ALU.add),
                       reads=[B["g"], bcst], writes=[B["b"]])
                    op("act", lambda h: h.activation(out=t_eb, in_=t_b, func=AF.Exp),
                       reads=[B["b"]], writes=[B["eb"]])
                    op("act", lambda h: h.activation(out=t_enb, in_=t_b, func=AF.Exp, scale=-1.0),
                       reads=[B["b"]], writes=[B["enb"]])
                    op("dve", lambda h, s=s: h.tensor_tensor(out=t_qe, in0=t_q[s], in1=t_eb, op=ALU.mult),
                       reads=[B["q%d" % s], B["eb"]], writes=[B["qe"]])
                    op("dve", lambda h: h.tensor_tensor(out=t_ke, in0=t_k, in1=t_enb, op=ALU.mult),
                       reads=[B["k"], B["enb"]], writes=[B["ke"]])
                    op("act", lambda h, hd=hd, s=s: h.activation(out=Sb[:, s, :], in_=Sf[:, hd, :], func=AF.Copy),
                       reads=[bS], writes=[bSb[s]])
                    po, bpo = ps[6], bps[6]
                    scan_chunks(hd, s, t_qe, t_ke, t_eb, (0, 1, 2, 3), mfw, po, bpo, True)
                    op("act", lambda h, po=po: h.activation(out=t_o, in_=po[:, :], func=AF.Copy),
                       reads=[bpo], writes=[B["o"]])
                    dma("sp", ofs[srow[s]:srow[s] + 128, :], t_o, B["o"], reads=[B["o"]])
            barrier()

        dma("sp", Sflat, sctx[:, :], bS, writes=[bS])
        op("dve", lambda h: h.tensor_scalar(out=Sflat, in0=Sflat, scalar1=minit[:, 1:2], scalar2=None, op0=ALU.mult),
           reads=[bS, bcst], writes=[bS])
        gfb = hT[:, 0:16, :].rearrange("p k n -> p (k n)").bitcast(F32)
        oT = hT
        for i in reversed(range(NT)):
            r0 = i * T
            for a in range(4):
                dma("sp", vtok[:, a, :], vs[r0 + a * 128:r0 + (a + 1) * 128, :], bx[a], writes=[bx[a]])
            pss, bpss = ps[5], bps[5]
            for hd in range(NH):
                srow = (i * NH + hd) * 128
                s = hd % 4
                dma("sp", t_q[0], qs[srow:srow + 128, :], B["q0"], writes=[B["q0"]])
                dma("sp", t_fb, fbs[srow:srow + 128, :], B["fb"], writes=[B["fb"]])
                dma("sp", t_o, ofs[srow:srow + 128, :], B["o"], writes=[B["o"]])
                op("act", lambda h: h.activation(out=t_g, in_=t_fb, func=AF.Ln), reads=[B["fb"]], writes=[B["g"]])
                op("dve", lambda h: h.tensor_scalar(out=t_k, in0=t_fb, scalar1=-1.0, scalar2=1.0,
                                                    op0=ALU.mult, op1=ALU.add), reads=[B["fb"]], writes=[B["k"]])
                op("dve", lambda h: h.tensor_tensor_scan(out=t_b, data0=rst[:, :], data1=t_g, initial=0.0,
                                                         op0=ALU.mult, op1=ALU.add),
                   reads=[B["g"], bcst], writes=[B["b"]])
                op("dve", lambda h: h.tensor_tensor(out=t_z, in0=t_g, in1=t_b, op=ALU.subtract),
                   reads=[B["g"], B["b"]], writes=[B["z"]])
                op("dve", lambda h: h.tensor_scalar(
                    out=t_ntot[:, 0:4], in0=t_b.rearrange("p (j n) -> p j n", j=4)[:, :, 127], scalar1=-1.0,
                    scalar2=None, op0=ALU.mult), reads=[B["b"]], writes=[B["ntot"]])
                for j in range(4):
                    cs = slice(j * 128, (j + 1) * 128)
                    op("act", lambda h, cs=cs, j=j: h.activation(out=t_eb[:, cs], in_=t_z[:, cs], func=AF.Exp,
                                                                 bias=t_b[:, j * 128 + 127:j * 128 + 128]),
                       reads=[B["z"], B["b"]], writes=[B["eb"]])
                    op("act", lambda h, cs=cs, j=j: h.activation(out=t_enb[:, cs], in_=t_z[:, cs], func=AF.Exp,
                                                                 scale=-1.0, bias=t_ntot[:, j:j + 1]),
                       reads=[B["z"], B["ntot"]], writes=[B["enb"]])
                op("dve", lambda h: h.tensor_tensor(out=t_qe, in0=t_q[0], in1=t_eb, op=ALU.mult),
                   reads=[B["q0"], B["eb"]], writes=[B["qe"]])
                op("dve", lambda h: h.tensor_tensor(out=t_ke, in0=t_k, in1=t_enb, op=ALU.mult),
                   reads=[B["k"], B["enb"]], writes=[B["ke"]])
                op("act", lambda h, hd=hd, s=s: h.activation(out=Sb[:, s, :], in_=Sf[:, hd, :], func=AF.Copy),
                   reads=[bS], writes=[bSb[s]])
                po, bpo = ps[6], bps[6]
                scan_chunks(hd, s, t_qe, t_ke, t_eb, (3, 2, 1, 0), mbw, po, bpo, False)
                op("dve", lambda h, po=po: h.tensor_tensor(out=t_o, in0=po[:, :], in1=t_o, op=ALU.add),
                   reads=[bpo, B["o"]], writes=[B["o"]])
                op("act", lambda h: h.activation(out=t_sq, in_=t_o, func=AF.Square), reads=[B["o"]],
                   writes=[B["sq"]])
                op("pe", lambda h, hd=hd: h.matmul(pss[:, :], ones, t_sq, start=(hd == 0), stop=(hd == NH - 1)),
                   reads=[B["sq"], bcst], writes=[bpss])
                op("dve", lambda h, hd=hd: h.tensor_copy(out=oT[:, hd, :], in_=t_o), reads=[B["o"]], writes=[bh])
            op("act", lambda h: h.activation(out=t_sq, in_=pss[:, :], func=AF.Ln, scale=1.0 / D, bias=epsb[:, 0:1]),
               reads=[bpss], writes=[B["sq"]])
            op("act", lambda h: h.activation(out=t_sq, in_=t_sq, func=AF.Exp, scale=-0.5),
               reads=[B["sq"]], writes=[B["sq"]])
            for hd in range(NH):
                srow = (i * NH + hd) * 128
                dma("sp", t_gate, gts[srow:srow + 128, :], B["gate"], writes=[B["gate"]])
                op("dve", lambda h, hd=hd: h.scalar_tensor_tensor(out=t_z, in0=oT[:, hd, :], scalar=vcol(4, hd),
                                                                  in1=t_sq, op0=ALU.mult, op1=ALU.mult),
                   reads=[bh, B["sq"], bcst], writes=[B["z"]])
                op("dve", lambda h, hd=hd: h.tensor_tensor(out=oT[:, hd, :], in0=t_z, in1=t_gate, op=ALU.mult),
                   reads=[B["z"], B["gate"]], writes=[bh])
            barrier()
            for a in range(4):
                dma("sp", xt[:, a, :], x1s[r0 + a * 128:r0 + (a + 1) * 128, :], bx[a], writes=[bx[a]])
            for ob in range(D // 512):
                def sink(a, p, bp, ob=ob):
                    xo = xt[:, a, ob * 512:(ob + 1) * 512]
                    op("dve", lambda h: h.tensor_tensor(out=xo, in0=p[:, :], in1=xo, op=ALU.add),
                       reads=[bp, bx[a]], writes=[bx[a]])
                tok_proj(w_out[:, ob * 512:(ob + 1) * 512], KC, lambda k, a: oT[:, k, a * 128:(a + 1) * 128],
                         [bh], sink)
            barrier()
            build_hT(3, junk)
            wv, bwv = wload(rt_d[:, :], KC, NE)
            for a in range(4):
                p, bp = nb()
                mm_group(p[:, 0:NE], bp, [(hT[:, kc, a * 128:(a + 1) * 128], wv[:, kc, :]) for kc in range(KC)],
                         reads=[bwv, bh])
                lg = rtmp[:, 0:8]
                op("dve", lambda h, p=p: h.tensor_copy(out=lg, in_=p[:, 0:NE]), reads=[bp], writes=[brt])
                op("dve", lambda h: h.max(out=rtmp[:, 8:16], in_=lg), reads=[brt], writes=[brt])
                op("dve", lambda h: h.tensor_tensor(out=rtmp[:, 16:17], in0=rtmp[:, 8:9], in1=rtmp[:, 9:10],
                                                    op=ALU.subtract), reads=[brt], writes=[brt])
                op("act", lambda h: h.activation(out=rtmp[:, 17:18], in_=rtmp[:, 16:17], func=AF.Sigmoid),
                   reads=[brt], writes=[brt])
                op("act", lambda h: h.activation(out=rtmp[:, 18:19], in_=rtmp[:, 16:17], func=AF.Sigmoid,
                                                 scale=-1.0), reads=[brt], writes=[brt])
                op("dve", lambda h: h.tensor_scalar(out=rtmp[:, 24:32], in0=lg, scalar1=rtmp[:, 8:9],
                                                    scalar2=rtmp[:, 17:18], op0=ALU.is_equal, op1=ALU.mult),
                   reads=[brt], writes=[brt])
                op("dve", lambda h: h.tensor_scalar(out=rtmp[:, 32:40], in0=lg, scalar1=rtmp[:, 9:10],
                                                    scalar2=rtmp[:, 18:19], op0=ALU.is_equal, op1=ALU.mult),
                   reads=[brt], writes=[brt])
                op("dve", lambda h, a=a: h.tensor_tensor(out=comb[:, a * 8:(a + 1) * 8], in0=rtmp[:, 24:32],
                                                         in1=rtmp[:, 32:40], op=ALU.add),
                   reads=[brt, bcomb], writes=[bcomb])
            for e in range(NE):
                swiglu_down(mg_d[e * D:(e + 1) * D, :], mu_d[e * D:(e + 1) * D, :], md_d[e * FE:(e + 1) * FE, :],
                            8, comb_col=e)
            barrier()
            dma("sp", gfb, gfin_d.partition_broadcast(128), bh, writes=[bh])
            for a in range(4):
                rms_stats(a, junk, a)
                op("dve", lambda h, a=a: h.scalar_tensor_tensor(out=xs, in0=xt[:, a, :], scalar=stat[:, a:a + 1],
                                                                in1=gfb, op0=ALU.mult, op1=ALU.mult),
                   reads=[bx[a], bstat, bh], writes=[bxs])
                dma("sp", y_d[r0 + a * 128:r0 + (a + 1) * 128, :], xs, bxs, reads=[bxs])
            barrier()
        barrier()
    return nc


_NC_CACHE = {}


def _pool_mats(pos_main, pos_halo, L):
    NT = pos_main.shape[0]
    pm = np.zeros((NT, 4, 4, 128, T), np.float32)
    ph = np.zeros((NT, 4, 16, T), np.float32)
    for i in range(NT):
        pt = pos_main[i].astype(np.int64)
        R = np.concatenate([pos_halo[i, 0:8], pos_main[i], pos_halo[i, 8:16]]).astype(np.int64)
        okr = (R >= 0) & (R < L)
        okt = (pt >= 0) & (pt < L)
        for g, w in enumerate(POOL_W):
            lo = np.clip(pt - w // 2, 0, L)
            hi = np.clip(pt + w // 2, 0, L)
            cnt = (hi - lo).astype(np.float32)
            inv = np.where(okt & (cnt > 0), 1.0 / np.maximum(cnt, 1.0), 0.0).astype(np.float32)
            M = ((R[:, None] >= lo[None, :]) & (R[:, None] < hi[None, :]) & okr[:, None]).astype(np.float32) * inv[None, :]
            M -= ((R[:, None] == pt[None, :]) & okt[None, :] & okr[:, None]).astype(np.float32)
            pm[i, g] = M[8:8 + T].reshape(4, 128, T)
            ph[i, g, 0:8] = M[0:8]
            ph[i, g, 8:16] = M[8 + T:]
    pm2 = np.ascontiguousarray(pm.transpose(0, 1, 3, 2, 4)).reshape(NT * 4 * 128, 4 * T)
    ph2 = np.ascontiguousarray(ph.transpose(0, 2, 1, 3)).reshape(NT * 16, 4 * T)
    return pm2.astype(ml_dtypes.bfloat16), ph2.astype(ml_dtypes.bfloat16)


def _consts():
    cst = np.zeros((128, 256), np.float32)
    cst[:, 0:128] = np.eye(128, dtype=np.float32)
    cst[:, 128:256] = 1.0
    cb = np.zeros((128, 512), np.float32)
    cb[:, 0:128] = np.eye(128)
    s = np.arange(128)[:, None]
    t = np.arange(128)[None, :]
    cb[:, 128:256] = (s <= t)
    cb[:, 256:384] = (s >= t)
    rst = np.ones((128, T), np.float32)
    rst[:, 0::128] = 0.0
    return cst, cb.astype(ml_dtypes.bfloat16), rst


def _vec_layout(v):
    return np.ascontiguousarray(np.asarray(v, np.float32).reshape(32, 128).T)


BIG = -(1 << 40)


def _gather(seq, pos):
    L = seq.shape[0]
    out = np.zeros((pos.size, D), np.float32)
    ok = (pos >= 0) & (pos < L)
    out[ok.reshape(-1)] = seq[pos.reshape(-1)[ok.reshape(-1)]]
    return out


def _tiles(pos):
    NT = pos.size // T
    main = pos.reshape(NT, T)
    halo = np.full((NT, 16), BIG, np.int64)
    for i in range(NT):
        m = main[i]
        if m[0] < 0:
            continue
        lo_, hi_ = m.min(), m.max()
        halo[i, 0:8] = lo_ - 8 + np.arange(8)
        halo[i, 8:16] = hi_ + 1 + np.arange(8)
    return main, halo


def _run(jobs, NT, NTC, norm_mix, norm_ffn, norm_final, pool_w, pool_scale, hgrn_w_in, hgrn_lb, hgrn_norm,
         hgrn_w_out, ffn_w_gate, ffn_w_up, ffn_w_down, moe_router, moe_w_gate, moe_w_up, moe_w_down):
    key = (NT, NTC)
    if key not in _NC_CACHE:
        _NC_CACHE[key] = build(NT, NTC)
    nc = _NC_CACHE[key]
    cst, cstb, rst = _consts()
    vec = np.zeros((128, 9 * 32), np.float32)
    for k, v in enumerate([norm_mix[0], norm_mix[1], norm_ffn[0], norm_ffn[1], hgrn_norm[0], hgrn_lb[0], hgrn_lb[1]]):
        vec[:, k * 32:(k + 1) * 32] = _vec_layout(v)
    w_in = np.ascontiguousarray(hgrn_w_in[0])
    wf = {1: np.ascontiguousarray(w_in[:, D:2 * D]), 2: np.ascontiguousarray(w_in[:, 2 * D:3 * D])}
    wf[0] = wf[1]
    shared = {
        "cst": cst, "cstb": cstb, "rst": rst, "vec": vec,
        "gfin": np.ascontiguousarray(norm_final, np.float32),
        "psc": np.ascontiguousarray(pool_scale[0], np.float32),
        "pool_w": np.ascontiguousarray(pool_w[0].reshape(4 * 1024, 1024)),
        "w_in": w_in, "w_out": np.ascontiguousarray(hgrn_w_out[0]),
        "wg": np.ascontiguousarray(ffn_w_gate[0]), "wu": np.ascontiguousarray(ffn_w_up[0]),
        "wd": np.ascontiguousarray(ffn_w_down[0]), "rt": np.ascontiguousarray(moe_router[0]),
        "mg": np.ascontiguousarray(moe_w_gate[0].reshape(NE * D, FE)),
        "mu": np.ascontiguousarray(moe_w_up[0].reshape(NE * D, FE)),
        "md": np.ascontiguousarray(moe_w_down[0].reshape(NE * FE, D)),
    }
    in_maps = []
    zero_seq = np.zeros((0, D), np.float32)
    cache = {}
    for seq, own_pos, ctx_pos, cdir in jobs:
        if seq is None:
            seq = zero_seq
        L = seq.shape[0]
        m = dict(shared)
        om, oh = _tiles(own_pos)
        cm, ch = _tiles(ctx_pos)
        m["x"] = _gather(seq, om)
        m["xh"] = _gather(seq, oh)
        m["xc"] = _gather(seq, cm)
        m["xhc"] = _gather(seq, ch)
        ko = ("o", L, own_pos[0], own_pos[-1])
        if ko not in cache:
            cache[ko] = _pool_mats(om, oh, L)
        m["pm"], m["ph"] = cache[ko]
        kc_ = ("c", L, ctx_pos[0], ctx_pos[-1])
        if kc_ not in cache:
            cache[kc_] = _pool_mats(cm, ch, L)
        m["pmc"], m["phc"] = cache[kc_]
        m["wfc"] = wf[cdir]
        mi = np.zeros((128, 2), np.float32)
        if cdir == 1:
            mi[:, 0] = 1.0
        elif cdir == 2:
            mi[:, 1] = 1.0
        m["minit"] = mi
        in_maps.append(m)
    res = run_bass_kernel_spmd(nc, in_maps, core_ids=list(range(8)))
    return [res.results[c]["y"] for c in range(8)]


def kernel(x_prompt, x_sample, **w):
    x_prompt = np.asarray(x_prompt)
    x_sample = np.asarray(x_sample)
    w = {k: np.asarray(v) for k, v in w.items()}
    LP = x_prompt.shape[1]
    LS = x_sample.shape[1]
    half = LP // 2
    NT = half // T
    NTC = half // T
    assert LS == half and x_sample.shape[0] == 4 and x_prompt.shape[0] == 1
    none_pos = np.full(NTC * T, BIG, np.int64)
    jobs = [
        (x_prompt[0], np.arange(0, half), np.arange(LP - 1, half - 1, -1), 2),
        (x_prompt[0], np.arange(half, LP), np.arange(0, half), 1),
    ]
    for b in range(4):
        jobs.append((x_sample[b], np.arange(0, LS), none_pos, 0))
    while len(jobs) < 8:
        jobs.append((None, np.full(NT * T, BIG, np.int64), none_pos, 0))
    ys = _run(jobs, NT, NTC, **w)
    yp = np.concatenate([ys[0], ys[1]], 0)[None]
    ysamp = np.stack([ys[2 + b] for b in range(4)], 0)
    return (np.ascontiguousarray(yp, dtype=np.float32), np.ascontiguousarray(ysamp, dtype=np.float32))
```
